# Optimizing a Trainium2 kernel written in Bass

```python
import jax
import jax.numpy as jnp
from jax import lax
import numpy as np

D_MODEL = 4096
BATCH = 4
SEQ = 2048
DEPTH = 2

RET_HEAD_DIM = 256
RET_WIDTH = D_MODEL // 2
RET_HEADS = RET_WIDTH // RET_HEAD_DIM
RET_CHUNK = 128
FOX_HEAD_DIM = 128
FOX_WIDTH = D_MODEL // 2
FOX_HEADS = FOX_WIDTH // FOX_HEAD_DIM
FOX_BLOCK = 128
LRU_WIDTH = D_MODEL // 2
LRU_BLOCKS = 16
LRU_BLOCK_DIM = LRU_WIDTH // LRU_BLOCKS
LRU_CONV_WIDTH = 4
LRU_C = 8.0
N_BRANCH = 3
D_FF = 2 * D_MODEL
HALF_STEP = 0.5
ROPE_BASE = 10000.0
EPS = 1e-6
IN_SIZES = (RET_WIDTH, RET_WIDTH, RET_WIDTH, RET_WIDTH,
            FOX_WIDTH, FOX_WIDTH, FOX_WIDTH, FOX_HEADS,
            LRU_WIDTH, LRU_WIDTH,
            N_BRANCH * D_MODEL)
N_IN = sum(IN_SIZES)

kernel_name = 'hybrid_retention_fox_rglru_macaron'


def rms_norm(x, g):
    xf = x.astype(jnp.float32)
    y = xf * lax.rsqrt(jnp.mean(xf * xf, axis=-1, keepdims=True) + EPS)
    return (y * g.astype(jnp.float32)).astype(x.dtype)


def swiglu(x, w_gate, w_up, w_down):
    return (jax.nn.silu(x @ w_gate) * (x @ w_up)) @ w_down


def apply_rotary(x, pos):
    half = x.shape[-1] // 2
    inv_freq = ROPE_BASE ** (-jnp.arange(half, dtype=jnp.float32) / half)
    ang = pos.astype(jnp.float32)[:, None] * inv_freq[None, :]
    cos = jnp.cos(ang)[None, :, None, :]
    sin = jnp.sin(ang)[None, :, None, :]
    xf = x.astype(jnp.float32)
    x1, x2 = xf[..., :half], xf[..., half:]
    return jnp.concatenate([x1 * cos - x2 * sin, x2 * cos + x1 * sin], axis=-1).astype(x.dtype)


def retention_chunkwise(q, k, v):
    B, S, H, d = q.shape
    C = RET_CHUNK
    n = S // C
    f32 = jnp.float32
    log_gamma = jnp.log(1.0 - jnp.exp2(-5.0 - jnp.arange(H, dtype=f32)))
    qc = q.astype(f32).reshape(B, n, C, H, d)
    kc = (k.astype(f32) * d ** -0.5).reshape(B, n, C, H, d)
    vc = v.astype(f32).reshape(B, n, C, H, d)
    idx = jnp.arange(C, dtype=f32)
    rel = idx[:, None] - idx[None, :]
    inner_decay = jnp.where(rel[None] >= 0,
                            jnp.exp(jnp.maximum(rel, 0.0)[None] * log_gamma[:, None, None]),
                            0.0)
    scores = jnp.einsum('bnihd,bnjhd->bnhij', qc, kc) * inner_decay
    inner = jnp.einsum('bnhij,bnjhe->bnihe', scores, vc)
    k_to_end = jnp.exp((C - 1.0 - idx)[:, None] * log_gamma[None, :])
    kv = jnp.einsum('bnjhd,bnjhe->bnhde', kc * k_to_end[:, :, None], vc)
    chunk_decay = jnp.exp(C * log_gamma)[:, None, None]

    def step(state, kv_n):
        return chunk_decay * state + kv_n, state

    _, prev = lax.scan(step, jnp.zeros((B, H, d, d), f32), jnp.moveaxis(kv, 1, 0))
    prev = jnp.moveaxis(prev, 0, 1)
    q_from_start = jnp.exp((idx + 1.0)[:, None] * log_gamma[None, :])
    cross = jnp.einsum('bnihd,bnhde->bnihe', qc * q_from_start[:, :, None], prev)
    return (inner + cross).reshape(B, S, H, d).astype(q.dtype)


def forgetting_attention(q, k, v, log_f):
    B, S, H, d = q.shape
    cum = jnp.cumsum(log_f, axis=1).transpose(0, 2, 1)
    scale = d ** -0.5
    outs = []
    for i in range(S // FOX_BLOCK):
        lo, hi = i * FOX_BLOCK, (i + 1) * FOX_BLOCK
        s = jnp.einsum('bqhd,bkhd->bhqk', q[:, lo:hi], k[:, :hi]).astype(jnp.float32) * scale
        bias = cum[:, :, lo:hi, None] - cum[:, :, None, :hi]
        causal = (lo + jnp.arange(FOX_BLOCK))[:, None] >= jnp.arange(hi)[None, :]
        p = jax.nn.softmax(jnp.where(causal, s + bias, -jnp.inf), axis=-1)
        outs.append(jnp.einsum('bhqk,bkhd->bqhd', p.astype(v.dtype), v[:, :hi]))
    return jnp.concatenate(outs, axis=1)


def causal_depthwise_conv(x, w, b):
    K = w.shape[0]
    S = x.shape[1]
    xp = jnp.pad(x, ((0, 0), (K - 1, 0), (0, 0)))
    return b + sum(xp[:, j:j + S] * w[j] for j in range(K))


def rg_lru(x, w_a, b_a, w_x, b_x, lam):
    B, S, W = x.shape
    xb = x.reshape(B, S, LRU_BLOCKS, LRU_BLOCK_DIM)
    r = jax.nn.sigmoid(jnp.einsum('bshi,hij->bshj', xb, w_a).reshape(B, S, W) + b_a)
    i = jax.nn.sigmoid(jnp.einsum('bshi,hij->bshj', xb, w_x).reshape(B, S, W) + b_x)
    log_a = -LRU_C * r.astype(jnp.float32) * jax.nn.softplus(-lam.astype(jnp.float32))
    a = jnp.exp(log_a)
    u = jnp.sqrt(-jnp.expm1(2.0 * log_a)) * (i.astype(jnp.float32) * x.astype(jnp.float32))

    def combine(left, right):
        a1, b1 = left
        a2, b2 = right
        return a1 * a2, a2 * b1 + b2

    _, h = lax.associative_scan(combine, (a, u), axis=1)
    return h.astype(x.dtype)


def setup_inputs(seed: int = 0) -> dict:
    key = jax.random.key(seed)
    ks = jax.random.split(key, 32)
    f32 = jnp.float32

    def dense(k, shape, fan_in):
        return jax.random.normal(k, shape, f32) * fan_in ** -0.5

    def gain(k, shape):
        return 1.0 + 0.02 * jax.random.normal(k, shape, f32)

    def small(k, shape, scale=0.01):
        return scale * jax.random.normal(k, shape, f32)

    u = jax.random.uniform(ks[17], (DEPTH, LRU_WIDTH), f32, 0.9, 0.999)
    s = u ** (1.0 / LRU_C)
    lru_lambda = jnp.log(s) - jnp.log1p(-s)
    return {
        'x': jax.random.normal(ks[0], (BATCH, SEQ, D_MODEL), f32),
        'ffn1_norm': gain(ks[1], (DEPTH, D_MODEL)),
        'ffn1_w_gate': dense(ks[2], (DEPTH, D_MODEL, D_FF), D_MODEL),
        'ffn1_w_up': dense(ks[3], (DEPTH, D_MODEL, D_FF), D_MODEL),
        'ffn1_w_down': dense(ks[4], (DEPTH, D_FF, D_MODEL), D_FF),
        'mix_norm': gain(ks[5], (DEPTH, D_MODEL)),
        'w_in': dense(ks[6], (DEPTH, D_MODEL, N_IN), D_MODEL),
        'ret_norm': gain(ks[7], (DEPTH, RET_HEADS, RET_HEAD_DIM)),
        'fox_q_norm': gain(ks[8], (DEPTH, FOX_HEAD_DIM)),
        'fox_k_norm': gain(ks[9], (DEPTH, FOX_HEAD_DIM)),
        'fox_f_bias': 2.0 + small(ks[10], (DEPTH, FOX_HEADS), 0.1),
        'lru_conv_w': dense(ks[11], (DEPTH, LRU_CONV_WIDTH, LRU_WIDTH), LRU_CONV_WIDTH),
        'lru_conv_b': small(ks[12], (DEPTH, LRU_WIDTH)),
        'lru_w_a': dense(ks[13], (DEPTH, LRU_BLOCKS, LRU_BLOCK_DIM, LRU_BLOCK_DIM), LRU_BLOCK_DIM),
        'lru_b_a': small(ks[14], (DEPTH, LRU_WIDTH)),
        'lru_w_x': dense(ks[15], (DEPTH, LRU_BLOCKS, LRU_BLOCK_DIM, LRU_BLOCK_DIM), LRU_BLOCK_DIM),
        'lru_b_x': small(ks[16], (DEPTH, LRU_WIDTH)),
        'lru_lambda': lru_lambda,
        'w_branch_ret': dense(ks[18], (DEPTH, RET_WIDTH, D_MODEL), RET_WIDTH),
        'w_branch_fox': dense(ks[19], (DEPTH, FOX_WIDTH, D_MODEL), FOX_WIDTH),
        'w_branch_lru': dense(ks[20], (DEPTH, LRU_WIDTH, D_MODEL), LRU_WIDTH),
        'w_out': dense(ks[21], (DEPTH, D_MODEL, D_MODEL), D_MODEL),
        'ffn2_norm': gain(ks[22], (DEPTH, D_MODEL)),
        'ffn2_w_gate': dense(ks[23], (DEPTH, D_MODEL, D_FF), D_MODEL),
        'ffn2_w_up': dense(ks[24], (DEPTH, D_MODEL, D_FF), D_MODEL),
        'ffn2_w_down': dense(ks[25], (DEPTH, D_FF, D_MODEL), D_FF),
    }


def reference(x, ffn1_norm, ffn1_w_gate, ffn1_w_up, ffn1_w_down, mix_norm, w_in, ret_norm,
              fox_q_norm, fox_k_norm, fox_f_bias, lru_conv_w, lru_conv_b, lru_w_a, lru_b_a,
              lru_w_x, lru_b_x, lru_lambda, w_branch_ret, w_branch_fox, w_branch_lru, w_out,
              ffn2_norm, ffn2_w_gate, ffn2_w_up, ffn2_w_down):
    B, S, _ = x.shape
    pos = jnp.arange(S)
    split_points = np.cumsum(IN_SIZES)[:-1].tolist()
    for l in range(DEPTH):
        x = x + HALF_STEP * swiglu(rms_norm(x, ffn1_norm[l]), ffn1_w_gate[l], ffn1_w_up[l], ffn1_w_down[l])

        h = rms_norm(x, mix_norm[l])
        proj = h @ w_in[l]
        (rq, rk, rv, rg, fq, fk, fv, fgate, lgate, lx, merge) = jnp.split(proj, split_points, axis=-1)

        rq = apply_rotary(rq.reshape(B, S, RET_HEADS, RET_HEAD_DIM), pos)
        rk = apply_rotary(rk.reshape(B, S, RET_HEADS, RET_HEAD_DIM), pos)
        ret = retention_chunkwise(rq, rk, rv.reshape(B, S, RET_HEADS, RET_HEAD_DIM))
        ret = jax.nn.silu(rg) * rms_norm(ret, ret_norm[l]).reshape(B, S, RET_WIDTH)

        fq = rms_norm(fq.reshape(B, S, FOX_HEADS, FOX_HEAD_DIM), fox_q_norm[l])
        fk = rms_norm(fk.reshape(B, S, FOX_HEADS, FOX_HEAD_DIM), fox_k_norm[l])
        log_f = jax.nn.log_sigmoid((fgate + fox_f_bias[l]).astype(jnp.float32))
        fox = forgetting_attention(fq, fk, fv.reshape(B, S, FOX_HEADS, FOX_HEAD_DIM), log_f)
        fox = fox.reshape(B, S, FOX_WIDTH)

        lx = causal_depthwise_conv(lx, lru_conv_w[l], lru_conv_b[l])
        lru = jax.nn.gelu(lgate) * rg_lru(lx, lru_w_a[l], lru_b_a[l], lru_w_x[l], lru_b_x[l], lru_lambda[l])

        g_ret, g_fox, g_lru = jnp.split(jax.nn.sigmoid(merge), N_BRANCH, axis=-1)
        mixed = (g_ret * (ret @ w_branch_ret[l])
                 + g_fox * (fox @ w_branch_fox[l])
                 + g_lru * (lru @ w_branch_lru[l]))
        x = x + mixed @ w_out[l]

        x = x + HALF_STEP * swiglu(rms_norm(x, ffn2_norm[l]), ffn2_w_gate[l], ffn2_w_up[l], ffn2_w_down[l])
    return x
```

```python
import contextlib
import math
import os
import numpy as np
import ml_dtypes
import concourse.bass as bass
import concourse.mybir as mybir
from concourse.bass_utils import run_bass_kernel_spmd

F32 = mybir.dt.float32
BF16 = mybir.dt.bfloat16
ALU = mybir.AluOpType
AF = mybir.ActivationFunctionType
NPBF = ml_dtypes.bfloat16

SEM_ROT = 30000
EPS = 1e-6
ROPE_BASE = 10000.0
LRU_C = 8.0
FOX_C = 16.0


class CFG:
    def __init__(self, D=4096, S=2048, B=4, DEPTH=2, NB=512, QR=512, AG=16, full=False):
        self.D, self.S, self.B, self.DEPTH = D, S, B, DEPTH
        self.full = full
        self.FF = 2 * D
        self.RW = self.FW = self.LW = D // 2
        self.RH = self.RW // 256
        self.FH = self.FW // 128
        self.LB = self.LW // 128
        dv = 1 if full else 2
        self.RHL, self.FHL, self.LBL = self.RH // dv, self.FH // dv, self.LB // dv
        self.TPC = S // 2
        self.NB = min(NB, self.TPC)
        self.NTB = self.TPC // self.NB
        self.QR = min(QR, S)
        self.DC = D // 128
        self.FFC = self.FF // 128
        self.AG = min(AG, self.FFC)
        self.FQ = self.FFC // self.AG
        self.BC = self.RW // 128
        self.BCL = self.BC // dv
        self.NCC = self.RHL * 8 + self.FHL * 3 + self.LBL * 2
        self.N_IN = 4 * self.RW + 3 * self.FW + self.FH + 2 * self.LW + 3 * D
        self.WSZ = max(self.DC, self.AG, self.BC) * 128


class _Buf:
    __slots__ = ("name", "w", "r", "sem", "semv")

    def __init__(self, name):
        self.name = name
        self.w = {}
        self.r = {}
        self.sem = None
        self.semv = 0


_BUFS = {}


def Buf(name):
    b = _BUFS.get(name)
    if b is None:
        b = _BUFS[name] = _Buf(name)
    return b


class Sched:
    def __init__(self, nc, stack):
        _BUFS.clear()
        self.nc = nc
        self.stack = stack
        self.engs = {"pe": nc.tensor, "act": nc.scalar, "dve": nc.vector, "pool": nc.gpsimd, "sp": nc.sync}
        self.esem = {}
        self.seen = {e: {} for e in self.engs}
        self.nsem = 0
        self.nwait = 0
        self.nins = {e: 0 for e in self.engs}
        self.dbufs = []
        self.allsems = []
        self.freesems = {}
        self.semq = {}
        self.persist = set()
        for e in self.engs:
            self.esem[e] = [self.new_sem(e), 0]

    def new_sem(self, tag):
        self.nsem += 1
        s = self.stack.enter_context(self.nc.semaphore(f"s{self.nsem}_{tag}"))
        return s

    def _deps(self, reads, writes):
        d = {}
        for b in reads:
            for k, sv in b.w.items():
                if k not in d or d[k][1] < sv[1]:
                    d[k] = sv
        for b in writes:
            for dd in (b.w, b.r):
                for k, sv in dd.items():
                    if k not in d or d[k][1] < sv[1]:
                        d[k] = sv
        return d

    def _wait(self, e, deps):
        seen = self.seen[e]
        own = id(self.esem[e][0])
        for k, (s, v) in deps.items():
            if e == "pe" and k == own:
                continue
            if seen.get(k, 0) >= v:
                continue
            self.engs[e].wait_ge(s, v)
            self.nwait += 1
            seen[k] = v

    def _record(self, rec, reads, writes):
        k = id(rec[0])
        for b in reads:
            b.r[k] = rec
        for b in writes:
            b.w[k] = rec

    def op(self, e, fn, reads=(), writes=(), inc=True):
        self._wait(e, self._deps(reads, writes))
        ins = fn(self.engs[e])
        self.nins[e] += 1
        st = self.esem[e]
        if inc:
            st[1] += 1
            ins.then_inc(st[0], 1)
            rec = (st[0], st[1])
            if st[1] >= SEM_ROT:
                self.allsems.append((st[0], st[1]))
                self.esem[e] = [self.new_sem(e), 0]
        else:
            rec = (st[0], st[1] + 1)
        self._record(rec, reads, writes)
        return ins

    def dma(self, q, out, in_, reads=(), writes=(), sem_buf=None):
        sb = sem_buf if sem_buf is not None else (writes[0] if writes else reads[0])
        if sb.sem is None:
            fl = self.freesems.get(q)
            if fl:
                sb.sem, sb.semv = fl.pop()
            else:
                sb.sem = self.new_sem("d" + sb.name)
                self.semq[id(sb.sem)] = q
            self.dbufs.append(sb)
        assert self.semq[id(sb.sem)] == q, (sb.name, q)
        self._wait(q, self._deps(reads, writes))
        ins = self.engs[q].dma_start(out=out, in_=in_)
        self.nins[q] += 1
        sb.semv += 16
        ins.then_inc(sb.sem, 16)
        rec = (sb.sem, sb.semv)
        self._record(rec, reads, writes)
        return ins

    def barrier(self, engines=("pe", "act", "dve", "pool", "sp")):
        d = {}
        for e, (s, v) in self.esem.items():
            if v > 0:
                d[id(s)] = (s, v)
        for s, v in self.allsems:
            d[id(s)] = (s, v)
        for b in self.dbufs:
            if b.semv > 0:
                d[id(b.sem)] = (b.sem, b.semv)
        for e in engines:
            self._wait(e, d)
        keep = []
        for b in self.dbufs:
            if True:
                keep.append(b)
            else:
                self.allsems.append((b.sem, b.semv))
                self.freesems.setdefault(self.semq[id(b.sem)], []).append((b.sem, b.semv))
                b.sem = None
        self.dbufs = keep


_UID = [0]


def T(pool_stack, nc, name, shape, dt):
    _UID[0] += 1
    return pool_stack.enter_context(nc.sbuf_tensor(f"{name}_u{_UID[0]}", list(shape), dt))


def PS(pool_stack, nc, name, shape, dt=F32):
    _UID[0] += 1
    return pool_stack.enter_context(nc.psum_tensor(f"{name}_u{_UID[0]}", list(shape), dt))


class GateFiller:
    def __init__(self, P, l, ps):
        c, nc = P.cfg, P.nc
        self.P, self.l = P, l
        self.h = T(ps, nc, "gf_h", [128, c.DC, c.TPC], BF16)
        self.hb = Buf("gf_h")
        self.g = [T(ps, nc, f"gf_g{i}", [128, c.NB], BF16) for i in range(2)]
        self.gb = [Buf(f"gf_g{i}") for i in range(2)]
        self.pf = [PS(ps, nc, f"gf_p{i}", [128, c.NB]) for i in range(2)]
        self.pfb = [Buf(f"gf_p{i}") for i in range(2)]
        self.gscb = Buf("gsc")
        self.gen = self._gen()

    def _gen(self):
        P, l = self.P, self.l
        c, S, D = P.cfg, P.S, P.Dr
        chunks = [(th, cc) for th in range(2) for cc in range(3 * c.DC)]
        pend = {}

        def issue(idx):
            cc = chunks[idx][1]
            pend[idx] = P.wload(D[f"wmrg_{l}"][cc], c.DC * 128)

        for i in range(min(2, len(chunks))):
            issue(i)
        k = 0
        for idx, (th, cc) in enumerate(chunks):
            if cc == 0:
                for i in range(c.DC):
                    S.dma("sp", self.h[:, i, :], D["hx"][th, i], writes=[self.hb])
            if idx + 2 < len(chunks):
                issue(idx + 2)
            w, wbf = pend.pop(idx)
            for tq in range(c.NTB):
                sl = slice(tq * c.NB, (tq + 1) * c.NB)
                a = k % 2
                k += 1
                for kc in range(c.DC):
                    S.op("pe", lambda e: e.matmul(self.pf[a][:], lhsT=w[:, kc * 128:(kc + 1) * 128], rhs=self.h[:, kc, sl],
                                                   start=(kc == 0), stop=(kc == c.DC - 1)),
                         reads=[wbf, self.hb], writes=[self.pfb[a]], inc=(kc == c.DC - 1))
                    if kc % 8 == 7 and kc != c.DC - 1:
                        yield
                S.op("act", lambda e: e.activation(out=self.g[a][:], in_=self.pf[a][:], func=AF.Sigmoid),
                     reads=[self.pfb[a]], writes=[self.gb[a]])
                S.dma("sp", D["gsc"][th, cc][:, sl], self.g[a][:], reads=[self.gb[a]], writes=[self.gscb])
                yield

    def fill(self, n):
        for _ in range(n):
            if next(self.gen, "done") == "done":
                return

    def drain(self):
        for _ in self.gen:
            pass

class Prog:
    def __init__(self, cfg, segments, layer_ids):
        self.cfg = c = cfg
        self.nc = nc = bass.Bass("TRN2", target_bir_lowering=False)
        self.segments = segments
        self.ext_in = {}
        self.ext_out = {}
        kinds = [k for k, _ in segments]
        self.kinds = kinds

    def din(self, name, shape, dt=F32):
        t = self.nc.dram_tensor(name, list(shape), dt, kind="ExternalInput").ap()
        self.ext_in[name] = (tuple(shape), dt)
        return t

    def dout(self, name, shape, dt=F32):
        t = self.nc.dram_tensor(name, list(shape), dt, kind="ExternalOutput").ap()
        self.ext_out[name] = (tuple(shape), dt)
        return t

    def dscr(self, name, shape, dt=F32):
        return self.nc.dram_tensor(name, list(shape), dt, kind="Internal").ap()

    def build(self):
        c, nc = self.cfg, self.nc
        kinds = self.kinds
        first, last = kinds[0], kinds[-1]
        has_tok = ("p1" in kinds) or ("p3" in kinds)
        with contextlib.ExitStack() as gs:
            self.gs = gs
            self.S = S = Sched(nc, gs)
            D = {}
            self.Dr = D
            if has_tok:
                D["x_in"] = self.din("x_in", [c.DC, 128, c.TPC])
                D["xs"] = self.dout("xs", [c.DC, 128, c.TPC])
            if "p1" in kinds:
                D["hx"] = self.dout("hx", [c.DC, 128, c.TPC], BF16)
            elif "p3" in kinds:
                D["hx"] = self.din("hx", [c.DC, 128, c.TPC], BF16)
            if "p2" in kinds:
                D["hfull"] = self.din("hfull", [2, c.DC, 128, c.TPC], BF16)
                D["abc_out"] = self.dout("abc_out", [3, c.BCL, 128, c.S], BF16)
                D["pz"] = self.dscr("pz", [c.NCC, 128, c.S])
                D["pf"] = self.dscr("pf", [c.FHL, c.S])
            if "p3" in kinds:
                D["abc_in"] = self.din("abc_in", [3, c.BC, 128, c.TPC], BF16)
                D["gsc"] = self.dscr("gsc", [3 * c.DC, 128, c.TPC], BF16)
            for kind, l in self.segments:
                if kind == "p1":
                    self._decl_ffn(D, f"f1_{l}")
                    D[f"n_mix_{l}"] = self.din(f"n_mix_{l}", [128, c.DC])
                elif kind == "p2":
                    D[f"wmix_{l}"] = self.din(f"wmix_{l}", [c.NCC, 128, c.DC * 128])
                    D[f"wfg_{l}"] = self.din(f"wfg_{l}", [128, c.DC * c.FHL])
                    D[f"retgn_{l}"] = self.din(f"retgn_{l}", [128, c.RHL * 2])
                    D[f"foxg_{l}"] = self.din(f"foxg_{l}", [128, 2])
                    D[f"foxb_{l}"] = self.din(f"foxb_{l}", [c.FHL, 1])
                    D[f"lrup_{l}"] = self.din(f"lrup_{l}", [128, c.LBL * 8])
                    D[f"lruw_{l}"] = self.din(f"lruw_{l}", [2 * c.LBL, 128, 128])
                elif kind == "p3":
                    D[f"wmrg_{l}"] = self.din(f"wmrg_{l}", [3 * c.DC, 128, c.DC * 128])
                    D[f"wbr_{l}"] = self.din(f"wbr_{l}", [3 * c.DC, 128, c.BC * 128])
                    D[f"wout_{l}"] = self.din(f"wout_{l}", [c.DC, 128, c.DC * 128])
                    self._decl_ffn(D, f"f2_{l}")
            if "p2" in kinds:
                D["cs"] = self.din("cs", [2, 128, c.S])
                D["rmask"] = self.din("rmask", [c.RHL, 128, 128])
                D["rvec"] = self.din("rvec", [128, c.RHL * 3])
                D["cb16"] = self.din("cb16", [128, 3 * 128], BF16)
                D["sel"] = self.din("sel", [c.FHL, c.FHL * 128], BF16)
                D["id8"] = self.din("id8", [c.FHL, c.FHL])
            D["ones32"] = self.din("ones32", [128, 128])

            self.NW = 5
            self.wsl = [T(gs, nc, f"wsl{i}", [128, c.WSZ], BF16) for i in range(self.NW)]
            self.wb = [Buf(f"w{i}") for i in range(self.NW)]
            self.wi = 0
            self.xt = [T(gs, nc, f"xt{i}", [128, c.TPC], F32) for i in range(3)]
            self.xtb = [Buf(f"xt{i}") for i in range(3)]
            self.xi = 0
            self.ones32 = T(gs, nc, "ones32", [128, 128], F32)
            self.cb = Buf("consts")
            S.dma("sp", self.ones32[:], D["ones32"], writes=[self.cb])
            self.xsb = [Buf(f"xs{i}") for i in range(c.DC)] if has_tok else []

            if has_tok:
                for i in range(c.DC):
                    S.dma("sp", D["xs"][i], D["x_in"][i], writes=[self.xsb[i]])
            for kind, l in self.segments:
                if kind == "p1":
                    self.ffn(f"f1_{l}")
                    self.mixnorm(l)
                elif kind == "p2":
                    self.phase2(l)
                elif kind == "p3":
                    self.phase3(l)
                    self.ffn(f"f2_{l}")
            S.barrier()
            print("prog", self.segments, "nins", S.nins, "nwait", S.nwait, "nsem", S.nsem)
        return nc


    def xs_ap(self, i):
        return self.Dr["xs"][self.th, i] if self.cfg.full else self.Dr["xs"][i]

    def hx_ap(self, i):
        return self.Dr["hx"][self.th, i] if self.cfg.full else self.Dr["hx"][i]

    def hfull_ap(self, th, i):
        return self.Dr["hx"][th, i] if self.cfg.full else self.Dr["hfull"][th, i]

    def abcin_ap(self, br, kk):
        c = self.cfg
        if c.full:
            return self.Dr["abc"][br, kk][:, self.th * c.TPC:(self.th + 1) * c.TPC]
        return self.Dr["abc_in"][br, kk]

    def abcout_ap(self, br, ch):
        return self.Dr["abc"][br, ch] if self.cfg.full else self.Dr["abc_out"][br, ch]

    def build_full(self):
        c, nc = self.cfg, self.nc
        with contextlib.ExitStack() as gs:
            self.gs = gs
            self.S = S = Sched(nc, gs)
            D = {}
            self.Dr = D
            D["x_in"] = self.din("x_in", [2, c.DC, 128, c.TPC])
            D["xs"] = self.dout("xs", [2, c.DC, 128, c.TPC])
            D["hx"] = self.dscr("hx", [2, c.DC, 128, c.TPC], BF16)
            D["abc"] = self.dscr("abc", [3, c.BC, 128, c.S], BF16)
            D["pz"] = self.dscr("pz", [c.NCC, 128, c.S])
            D["pf"] = self.dscr("pf", [c.FHL, c.S])
            D["gsc"] = self.dscr("gsc", [2, 3 * c.DC, 128, c.TPC], BF16)
            for l in range(c.DEPTH):
                self._decl_ffn(D, f"f1_{l}")
                D[f"n_mix_{l}"] = self.din(f"n_mix_{l}", [128, c.DC])
                D[f"wmix_{l}"] = self.din(f"wmix_{l}", [c.NCC, 128, c.DC * 128])
                D[f"wfg_{l}"] = self.din(f"wfg_{l}", [128, c.DC * c.FHL])
                D[f"retgn_{l}"] = self.din(f"retgn_{l}", [128, c.RHL * 2])
                D[f"foxg_{l}"] = self.din(f"foxg_{l}", [128, 2])
                D[f"foxb_{l}"] = self.din(f"foxb_{l}", [c.FHL, 1])
                D[f"lrup_{l}"] = self.din(f"lrup_{l}", [128, c.LBL * 8])
                D[f"lruw_{l}"] = self.din(f"lruw_{l}", [2 * c.LBL, 128, 128])
                D[f"wmrg_{l}"] = self.din(f"wmrg_{l}", [3 * c.DC, 128, c.DC * 128])
                D[f"wbr_{l}"] = self.din(f"wbr_{l}", [3 * c.DC, 128, c.BC * 128])
                D[f"wout_{l}"] = self.din(f"wout_{l}", [c.DC, 128, c.DC * 128])
                self._decl_ffn(D, f"f2_{l}")
            D["cs"] = self.din("cs", [2, 128, c.S])
            D["rmask"] = self.din("rmask", [c.RHL, 128, 128])
            D["rvec"] = self.din("rvec", [128, c.RHL * 3])
            D["cb16"] = self.din("cb16", [128, 3 * 128], BF16)
            D["sel"] = self.din("sel", [c.FHL, c.FHL * 128], BF16)
            D["id8"] = self.din("id8", [c.FHL, c.FHL])
            D["ones32"] = self.din("ones32", [128, 128])
            self.NW = 5
            self.wsl = [T(gs, nc, f"wsl{i}", [128, c.WSZ], BF16) for i in range(self.NW)]
            self.wb = [Buf(f"w{i}") for i in range(self.NW)]
            self.wi = 0
            self.xt = [T(gs, nc, f"xt{i}", [128, c.TPC], F32) for i in range(3)]
            self.xtb = [Buf(f"xt{i}") for i in range(3)]
            self.xi = 0
            self.ones32 = T(gs, nc, "ones32", [128, 128], F32)
            self.cb = Buf("consts")
            S.dma("sp", self.ones32[:], D["ones32"], writes=[self.cb])
            self.xsb = [Buf(f"xs{i}") for i in range(c.DC)]
            for th in range(2):
                for i in range(c.DC):
                    S.dma("sp", D["xs"][th, i], D["x_in"][th, i], writes=[self.xsb[i]])
            if os.environ.get("B0", "1") == "1":
                S.barrier()
            nst = int(os.environ.get("FUSE_STAGES", "99"))
            st = 0
            for l in range(c.DEPTH):
                for th in range(2):
                    self.th = th
                    if st < nst:
                        self.ffn(f"f1_{l}")
                    st += 1
                    if st < nst:
                        self.mixnorm(l)
                    st += 1
                if st < nst:
                    self.phase2(l)
                st += 1
                for th in range(2):
                    self.th = th
                    if st < nst:
                        self.phase3(l)
                    st += 1
                    if st < nst:
                        self.ffn(f"f2_{l}")
                    st += 1
            S.barrier()
            print("prog full nins", S.nins, "nwait", S.nwait, "nsem", S.nsem)
        return nc

    filler = None

    def fill_hook(self, n):
        if self.filler is not None:
            self.filler.fill(n)

    def gsc_ap(self, idx):
        return self.Dr["gsc"][self.th, idx] if self.cfg.full else self.Dr["gsc"][idx]

    def _decl_ffn(self, D, p):
        c = self.cfg
        D[p + "_n"] = self.din(p + "_n", [128, c.DC])
        D[p + "_wg"] = self.din(p + "_wg", [c.FFC, 128, c.DC * 128])
        D[p + "_wu"] = self.din(p + "_wu", [c.FFC, 128, c.DC * 128])
        D[p + "_wd"] = self.din(p + "_wd", [c.FQ * c.DC, 128, c.AG * 128])

    def wload(self, src, width):
        i = self.wi
        self.wi = (i + 1) % self.NW
        self.S.dma("pool", self.wsl[i][:, :width], src, writes=[self.wb[i]])
        return self.wsl[i], self.wb[i]

    def xslot(self):
        i = self.xi
        self.xi = (i + 1) % 3
        return self.xt[i], self.xtb[i]

    def rmsnorm_fm(self, ps, gname, out16, outb):
        c, S, nc, D = self.cfg, self.S, self.nc, self.Dr
        gt = T(ps, nc, "nrm_g", [128, c.DC], F32)
        gb = Buf("nrm_g")
        S.dma("sp", gt[:], D[gname], writes=[gb])
        sq = [T(ps, nc, f"nrm_sq{i}", [128, c.TPC], F32) for i in range(2)]
        sqb = [Buf(f"nrm_sq{i}") for i in range(2)]
        rstd = T(ps, nc, "nrm_rstd", [128, c.TPC], F32)
        rb = Buf("nrm_rstd")
        pss = [PS(ps, nc, f"nrm_ps{t}", [128, c.NB]) for t in range(c.NTB)]
        pssb = [Buf(f"nrm_ps{t}") for t in range(c.NTB)]
        for i in range(c.DC):
            xt, xb = self.xslot()
            S.dma("sp", xt[:], self.xs_ap(i), reads=[self.xsb[i]], writes=[xb])
            s = i % 2
            S.op("act", lambda e: e.activation(out=sq[s][:], in_=xt[:], func=AF.Square), reads=[xb], writes=[sqb[s]])
            for t in range(c.NTB):
                S.op("pe", lambda e: e.matmul(pss[t][:], lhsT=self.ones32[:], rhs=sq[s][:, t * c.NB:(t + 1) * c.NB],
                                               start=(i == 0), stop=(i == c.DC - 1)),
                     reads=[sqb[s], self.cb], writes=[pssb[t]], inc=True)
        for t in range(c.NTB):
            sl = slice(t * c.NB, (t + 1) * c.NB)
            S.op("dve", lambda e: e.tensor_scalar(out=rstd[:, sl], in0=pss[t][:], scalar1=1.0 / c.D, scalar2=EPS,
                                                  op0=ALU.mult, op1=ALU.add), reads=[pssb[t]], writes=[rb])
        S.op("act", lambda e: e.activation(out=rstd[:], in_=rstd[:], func=AF.Sqrt), reads=[rb], writes=[rb])
        S.op("dve", lambda e: e.reciprocal(out=rstd[:], in_=rstd[:]), reads=[rb], writes=[rb])
        for i in range(c.DC):
            xt, xb = self.xslot()
            S.dma("sp", xt[:], self.xs_ap(i), reads=[self.xsb[i]], writes=[xb])
            S.op("dve", lambda e: e.scalar_tensor_tensor(out=out16[:, i, :], in0=xt[:], scalar=gt[:, i:i + 1],
                                                         in1=rstd[:], op0=ALU.mult, op1=ALU.mult),
                 reads=[xb, gb, rb], writes=[outb])

    def ffn(self, p):
        c, S, nc, D = self.cfg, self.S, self.nc, self.Dr
        with contextlib.ExitStack() as ps:
            h16 = T(ps, nc, "ffn_h", [128, c.DC, c.TPC], BF16)
            hb = Buf("ffn_h")
            with contextlib.ExitStack() as ps2:
                self.rmsnorm_fm(ps2, p + "_n", h16, hb)
                S.barrier()
            act = T(ps, nc, "ffn_act", [128, c.AG, c.TPC], BF16)
            actb = Buf("ffn_act")
            sg = [T(ps, nc, f"ffn_sg{i}", [128, c.NB], F32) for i in range(2)]
            sgb = [Buf(f"ffn_sg{i}") for i in range(2)]
            npb = 2 if 4 * c.NTB <= 8 else 1
            pg = [[PS(ps, nc, f"ffn_pg{a}_{t}", [128, c.NB]) for t in range(c.NTB)] for a in range(npb)]
            pu = [[PS(ps, nc, f"ffn_pu{a}_{t}", [128, c.NB]) for t in range(c.NTB)] for a in range(npb)]
            pgb = [[Buf(f"pg{a}_{t}") for t in range(c.NTB)] for a in range(npb)]
            pub = [[Buf(f"pu{a}_{t}") for t in range(c.NTB)] for a in range(npb)]
            k = 0
            for q in range(c.FQ):
                for jj in range(c.AG):
                    j = q * c.AG + jj
                    a = j % npb
                    wg, wgb = self.wload(D[p + "_wg"][j], c.DC * 128)
                    wu, wub = self.wload(D[p + "_wu"][j], c.DC * 128)
                    for t in range(c.NTB):
                        sl = slice(t * c.NB, (t + 1) * c.NB)
                        for kc in range(c.DC):
                            S.op("pe", lambda e: e.matmul(pg[a][t][:], lhsT=wg[:, kc * 128:(kc + 1) * 128], rhs=h16[:, kc, sl],
                                                           start=(kc == 0), stop=(kc == c.DC - 1)),
                                 reads=[wgb, hb], writes=[pgb[a][t]], inc=(kc == c.DC - 1))
                        for kc in range(c.DC):
                            S.op("pe", lambda e: e.matmul(pu[a][t][:], lhsT=wu[:, kc * 128:(kc + 1) * 128], rhs=h16[:, kc, sl],
                                                           start=(kc == 0), stop=(kc == c.DC - 1)),
                                 reads=[wub, hb], writes=[pub[a][t]], inc=(kc == c.DC - 1))
                        s = k % 2
                        k += 1
                        S.op("act", lambda e: e.activation(out=sg[s][:], in_=pg[a][t][:], func=AF.Silu),
                             reads=[pgb[a][t]], writes=[sgb[s]])
                        S.op("dve", lambda e: e.tensor_tensor(out=act[:, jj, sl], in0=sg[s][:], in1=pu[a][t][:], op=ALU.mult),
                             reads=[sgb[s], pub[a][t]], writes=[actb])
                for i in range(c.DC):
                    a = i % npb
                    wd, wdb = self.wload(D[p + "_wd"][q * c.DC + i], c.AG * 128)
                    xt, xb = self.xslot()
                    S.dma("sp", xt[:], self.xs_ap(i), reads=[self.xsb[i]], writes=[xb])
                    for t in range(c.NTB):
                        sl = slice(t * c.NB, (t + 1) * c.NB)
                        for kk in range(c.AG):
                            S.op("pe", lambda e: e.matmul(pg[a][t][:], lhsT=wd[:, kk * 128:(kk + 1) * 128], rhs=act[:, kk, sl],
                                                           start=(kk == 0), stop=(kk == c.AG - 1)),
                                 reads=[wdb, actb], writes=[pgb[a][t]], inc=(kk == c.AG - 1))
                        S.op("dve", lambda e: e.scalar_tensor_tensor(out=xt[:, sl], in0=pg[a][t][:], scalar=0.5, in1=xt[:, sl],
                                                                     op0=ALU.mult, op1=ALU.add),
                             reads=[pgb[a][t], xb], writes=[xb])
                    S.dma("sp", self.xs_ap(i), xt[:], reads=[xb], writes=[self.xsb[i]])
            S.barrier()

    def mixnorm(self, l):
        c, S, nc, D = self.cfg, self.S, self.nc, self.Dr
        with contextlib.ExitStack() as ps:
            h16 = T(ps, nc, "mn_h", [128, c.DC, c.TPC], BF16)
            hb = Buf("mn_h")
            self.rmsnorm_fm(ps, f"n_mix_{l}", h16, hb)
            hxb = Buf("hx")
            for i in range(c.DC):
                S.dma("sp", self.hx_ap(i), h16[:, i, :], reads=[hb], writes=[hxb])
            S.barrier()

    def phase2(self, l):
        c, S, nc, D = self.cfg, self.S, self.nc, self.Dr
        pzb = Buf("pz")
        pfb = Buf("pf")
        with contextlib.ExitStack() as ps:
            h16 = T(ps, nc, "p2_h", [128, c.DC, c.TPC], BF16)
            hb = Buf("p2_h")
            wfg = T(ps, nc, "p2_wfg", [128, c.DC * c.FHL], BF16)
            wfgb = Buf("p2_wfg")
            S.dma("pool", wfg[:], D[f"wfg_{l}"], writes=[wfgb])
            st = [T(ps, nc, f"p2_st{i}", [128, c.TPC], F32) for i in range(2)]
            stb = [Buf(f"p2_st{i}") for i in range(2)]
            stf = T(ps, nc, "p2_stf", [c.FHL, c.TPC], F32)
            stfb = Buf("p2_stf")
            pp = [[PS(ps, nc, f"p2_pp{a}_{t}", [128, c.NB]) for t in range(c.NTB)] for a in range(2)]
            ppb = [[Buf(f"p2pp{a}_{t}") for t in range(c.NTB)] for a in range(2)]
            for th in range(2):
                for i in range(c.DC):
                    S.dma("sp", h16[:, i, :], self.hfull_ap(th, i), writes=[hb])
                for cc in range(c.NCC):
                    a = cc % 2
                    w, wbf = self.wload(D[f"wmix_{l}"][cc], c.DC * 128)
                    for t in range(c.NTB):
                        sl = slice(t * c.NB, (t + 1) * c.NB)
                        for kc in range(c.DC):
                            S.op("pe", lambda e: e.matmul(pp[a][t][:], lhsT=w[:, kc * 128:(kc + 1) * 128], rhs=h16[:, kc, sl],
                                                           start=(kc == 0), stop=(kc == c.DC - 1)),
                                 reads=[wbf, hb], writes=[ppb[a][t]], inc=(kc == c.DC - 1))
                        S.op("act", lambda e: e.copy(out=st[a][:, sl], in_=pp[a][t][:]), reads=[ppb[a][t]], writes=[stb[a]])
                    S.dma("sp", D["pz"][cc, :, th * c.TPC:(th + 1) * c.TPC], st[a][:], reads=[stb[a]], writes=[pzb])
                for t in range(c.NTB):
                    sl = slice(t * c.NB, (t + 1) * c.NB)
                    for kc in range(c.DC):
                        S.op("pe", lambda e: e.matmul(pp[0][t][:c.FHL, :], lhsT=wfg[:, kc * c.FHL:(kc + 1) * c.FHL], rhs=h16[:, kc, sl],
                                                       start=(kc == 0), stop=(kc == c.DC - 1)),
                             reads=[wfgb, hb], writes=[ppb[0][t]], inc=(kc == c.DC - 1))
                    S.op("act", lambda e: e.copy(out=stf[:, sl], in_=pp[0][t][:c.FHL, :]), reads=[ppb[0][t]], writes=[stfb])
                S.dma("sp", D["pf"][:, th * c.TPC:(th + 1) * c.TPC], stf[:], reads=[stfb], writes=[pfb])
            S.barrier()
        abcb = Buf("abc_out")
        with contextlib.ExitStack() as ps:
            cb16 = T(ps, nc, "cb16", [128, 3 * 128], BF16)
            kb = Buf("cb16")
            S.dma("sp", cb16[:], D["cb16"], writes=[kb])
            self.tri, self.ident, self.ones16, self.c16b = cb16[:, 0:128], cb16[:, 128:256], cb16[:, 256:384], kb
            skip = os.environ.get("SKIPMIX", "")
            if "r" not in skip:
                self.retention(l, ps, pzb, abcb)
            with contextlib.ExitStack() as fs:
                if c.full and os.environ.get("NOFILL", "0") != "1":
                    self.filler = GateFiller(self, l, fs)
                if "f" not in skip:
                    self.fox(l, ps, pzb, pfb, abcb)
                if "l" not in skip:
                    self.lru(l, ps, pzb, abcb)
                if self.filler is not None:
                    self.filler.drain()
                    S.barrier()
                    self.filler = None
                    self.gates_done = True

    def retention(self, l, gps, pzb, abcb):
        c, S, nc, D = self.cfg, self.S, self.nc, self.Dr
        NCH = c.S // 128
        with contextlib.ExitStack() as ps:
            cs = T(ps, nc, "r_cs", [128, 2, c.S], F32)
            rmask = T(ps, nc, "r_mask", [128, c.RHL, 128], F32)
            rvec = T(ps, nc, "r_vec", [128, c.RHL * 3], F32)
            gn = T(ps, nc, "r_gn", [128, c.RHL * 2], F32)
            kb = Buf("r_consts")
            S.dma("sp", cs[:, 0, :], D["cs"][0], writes=[kb])
            S.dma("sp", cs[:, 1, :], D["cs"][1], writes=[kb])
            for h in range(c.RHL):
                S.dma("sp", rmask[:, h, :], D["rmask"][h], writes=[kb])
            S.dma("sp", rvec[:], D["rvec"], writes=[kb])
            S.dma("sp", gn[:], D[f"retgn_{l}"], writes=[kb])
            q32 = T(ps, nc, "r_q32", [128, 2, c.S], F32); q32b = Buf("r_q32")
            k32 = T(ps, nc, "r_k32", [128, 2, c.S], F32); k32b = Buf("r_k32")
            v16 = T(ps, nc, "r_v16", [128, 2, c.S], BF16); v16b = Buf("r_v16")
            qr = T(ps, nc, "r_qr", [128, 2, c.S], BF16); qrb = Buf("r_qr")
            kr = T(ps, nc, "r_kr", [128, 2, c.S], BF16); krb = Buf("r_kr")
            sg16 = T(ps, nc, "r_sg", [128, 2, c.S], BF16); sgb = Buf("r_sg")
            A16 = T(ps, nc, "r_A", [128, 2, c.S], BF16); Ab = Buf("r_A")
            t1 = T(ps, nc, "r_t1", [128, c.S], F32); t1b = Buf("r_t1")
            t2 = T(ps, nc, "r_t2", [128, c.S], F32); t2b = Buf("r_t2")
            st32 = T(ps, nc, "r_st32", [128, 2, 256], F32); st32b = Buf("r_st32")
            st16 = T(ps, nc, "r_st16", [128, 2, 256], BF16); st16b = Buf("r_st16")
            sm16 = T(ps, nc, "r_sm16", [128, 128], BF16); smb = Buf("r_sm16")
            kd16 = T(ps, nc, "r_kd16", [128, 256], BF16); kdb = Buf("r_kd16")
            vT16 = T(ps, nc, "r_vT16", [128, 256], BF16); vTb = Buf("r_vT16")
            in32 = T(ps, nc, "r_in32", [128, 256], F32); inb = Buf("r_in32")
            o32 = T(ps, nc, "r_o32", [128, 256], F32); ob = Buf("r_o32")
            junk = T(ps, nc, "r_junk", [128, 256], F32); jb = Buf("r_junk")
            ss = T(ps, nc, "r_ss", [128, 1], F32); ssb = Buf("r_ss")
            y16 = T(ps, nc, "r_y16", [128, 256], BF16); yb = Buf("r_y16")
            p_s = PS(ps, nc, "r_ps", [128, 128]); p_sb = Buf("r_ps")
            p_kT = PS(ps, nc, "r_pkT", [128, 256], BF16); p_kTb = Buf("r_pkT")
            p_vT = PS(ps, nc, "r_pvT", [128, 256], BF16); p_vTb = Buf("r_pvT")
            p_o = PS(ps, nc, "r_po", [128, 256]); p_ob = Buf("r_po")
            p_c = PS(ps, nc, "r_pc", [128, 256]); p_cb = Buf("r_pc")
            p_kv = PS(ps, nc, "r_pkv", [128, 2, 256]); p_kvb = Buf("r_pkv")
            p_yT = PS(ps, nc, "r_pyT", [128, 2, 128], BF16); p_yTb = Buf("r_pyT")
            cos, sin = cs[:, 0, :], cs[:, 1, :]
            for h in range(c.RHL):
                base = h * 8
                for cc in range(2):
                    S.dma("sp", q32[:, cc, :], D["pz"][base + cc], reads=[pzb], writes=[q32b])
                    S.dma("sp", k32[:, cc, :], D["pz"][base + 2 + cc], reads=[pzb], writes=[k32b])
                    S.dma("pool", v16[:, cc, :], D["pz"][base + 4 + cc], reads=[pzb], writes=[v16b])
                for (src, srcb, dst, dstb) in ((q32, q32b, qr, qrb), (k32, k32b, kr, krb)):
                    S.op("dve", lambda e: e.tensor_tensor(out=t1[:], in0=src[:, 0, :], in1=cos, op=ALU.mult), reads=[srcb, kb], writes=[t1b])
                    S.op("pool", lambda e: e.tensor_tensor(out=t2[:], in0=src[:, 1, :], in1=sin, op=ALU.mult), reads=[srcb, kb], writes=[t2b])
                    S.op("dve", lambda e: e.tensor_tensor(out=dst[:, 0, :], in0=t1[:], in1=t2[:], op=ALU.subtract), reads=[t1b, t2b], writes=[dstb])
                    S.op("dve", lambda e: e.tensor_tensor(out=t1[:], in0=src[:, 1, :], in1=cos, op=ALU.mult), reads=[srcb, kb], writes=[t1b])
                    S.op("pool", lambda e: e.tensor_tensor(out=t2[:], in0=src[:, 0, :], in1=sin, op=ALU.mult), reads=[srcb, kb], writes=[t2b])
                    S.op("dve", lambda e: e.tensor_tensor(out=dst[:, 1, :], in0=t1[:], in1=t2[:], op=ALU.add), reads=[t1b, t2b], writes=[dstb])
                for cc in range(2):
                    S.dma("sp", t1[:], D["pz"][base + 6 + cc], reads=[pzb], writes=[t1b])
                    S.op("act", lambda e: e.activation(out=sg16[:, cc, :], in_=t1[:], func=AF.Silu), reads=[t1b], writes=[sgb])
                kdec, dq, gC = rvec[:, 3 * h:3 * h + 1], rvec[:, 3 * h + 1:3 * h + 2], rvec[:, 3 * h + 2:3 * h + 3]
                for n in range(NCH):
                    tk = slice(n * 128, (n + 1) * 128)
                    for cc in range(2):
                        S.op("pe", lambda e: e.matmul(p_s[:], lhsT=kr[:, cc, tk], rhs=qr[:, cc, tk], start=(cc == 0), stop=(cc == 1)),
                             reads=[krb, qrb], writes=[p_sb], inc=(cc == 1))
                    S.op("dve", lambda e: e.tensor_tensor(out=sm16[:], in0=p_s[:], in1=rmask[:, h, :], op=ALU.mult),
                         reads=[p_sb, kb], writes=[smb])
                    for cc in range(2):
                        S.op("pe", lambda e: e.transpose(p_kT[:, cc * 128:(cc + 1) * 128], kr[:, cc, tk], self.ident),
                             reads=[krb, self.c16b], writes=[p_kTb], inc=(cc == 1))
                    S.op("dve", lambda e: e.tensor_scalar(out=kd16[:], in0=p_kT[:], scalar1=kdec, scalar2=None, op0=ALU.mult),
                         reads=[p_kTb, kb], writes=[kdb])
                    for cc in range(2):
                        S.op("pe", lambda e: e.transpose(p_vT[:, cc * 128:(cc + 1) * 128], v16[:, cc, tk], self.ident),
                             reads=[v16b, self.c16b], writes=[p_vTb], inc=(cc == 1))
                    S.op("act", lambda e: e.copy(out=vT16[:], in_=p_vT[:]), reads=[p_vTb], writes=[vTb])
                    S.op("pe", lambda e: e.matmul(p_o[:], lhsT=sm16[:], rhs=vT16[:], start=True, stop=True),
                         reads=[smb, vTb], writes=[p_ob], inc=True)
                    if n > 0:
                        for cc in range(2):
                            S.op("pe", lambda e: e.matmul(p_c[:], lhsT=qr[:, cc, tk], rhs=st16[:, cc, :], start=(cc == 0), stop=(cc == 1)),
                                 reads=[qrb, st16b], writes=[p_cb], inc=(cc == 1))
                        S.op("act", lambda e: e.copy(out=in32[:], in_=p_o[:]), reads=[p_ob], writes=[inb])
                        S.op("dve", lambda e: e.scalar_tensor_tensor(out=o32[:], in0=p_c[:], scalar=dq, in1=in32[:], op0=ALU.mult, op1=ALU.add),
                             reads=[p_cb, inb, kb], writes=[ob])
                    else:
                        S.op("act", lambda e: e.copy(out=o32[:], in_=p_o[:]), reads=[p_ob], writes=[ob])
                    if n < NCH - 1:
                        for cc in range(2):
                            S.op("pe", lambda e: e.matmul(p_kv[:, cc, :], lhsT=kd16[:, cc * 128:(cc + 1) * 128], rhs=vT16[:], start=True, stop=True),
                                 reads=[kdb, vTb], writes=[p_kvb], inc=(cc == 1))
                        if n == 0:
                            S.op("dve", lambda e: e.tensor_copy(out=st32[:], in_=p_kv[:]), reads=[p_kvb], writes=[st32b])
                        else:
                            for cc in range(2):
                                S.op("dve", lambda e: e.scalar_tensor_tensor(out=st32[:, cc, :], in0=st32[:, cc, :], scalar=gC, in1=p_kv[:, cc, :],
                                                                             op0=ALU.mult, op1=ALU.add),
                                     reads=[p_kvb, st32b, kb], writes=[st32b])
                        S.op("pool", lambda e: e.tensor_copy(out=st16[:], in_=st32[:]), reads=[st32b], writes=[st16b])
                    S.op("act", lambda e: e.activation(out=junk[:], in_=o32[:], func=AF.Square, accum_out=ss[:]), reads=[ob], writes=[jb, ssb])
                    S.op("dve", lambda e: e.tensor_scalar(out=ss[:], in0=ss[:], scalar1=1.0 / 256, scalar2=EPS, op0=ALU.mult, op1=ALU.add),
                         reads=[ssb], writes=[ssb])
                    S.op("act", lambda e: e.activation(out=ss[:], in_=ss[:], func=AF.Sqrt), reads=[ssb], writes=[ssb])
                    S.op("dve", lambda e: e.reciprocal(out=ss[:], in_=ss[:]), reads=[ssb], writes=[ssb])
                    S.op("dve", lambda e: e.tensor_scalar(out=y16[:], in0=o32[:], scalar1=ss[:, 0:1], scalar2=None, op0=ALU.mult),
                         reads=[ob, ssb], writes=[yb])
                    for cc in range(2):
                        S.op("pe", lambda e: e.transpose(p_yT[:, cc, :], y16[:, cc * 128:(cc + 1) * 128], self.ident),
                             reads=[yb, self.c16b], writes=[p_yTb], inc=(cc == 1))
                    for cc in range(2):
                        S.op("dve", lambda e: e.scalar_tensor_tensor(out=A16[:, cc, tk], in0=p_yT[:, cc, :], scalar=gn[:, 2 * h + cc:2 * h + cc + 1],
                                                                     in1=sg16[:, cc, tk], op0=ALU.mult, op1=ALU.mult),
                             reads=[p_yTb, sgb, kb], writes=[Ab])
                for cc in range(2):
                    S.dma("sp", self.abcout_ap(0, h * 2 + cc), A16[:, cc, :], reads=[Ab], writes=[abcb])
            S.barrier()

    def fox(self, l, gps, pzb, pfb, abcb):
        c, S, nc, D = self.cfg, self.S, self.nc, self.Dr
        NJ = c.S // 128
        NR = c.S // c.QR
        JR = c.QR // 128
        H = c.FHL
        with contextlib.ExitStack() as ps:
            kb = Buf("f_consts")
            sel = T(ps, nc, "f_sel", [H, H * 128], BF16)
            id8 = T(ps, nc, "f_id8", [H, H], F32)
            fg = T(ps, nc, "f_g", [128, 2], F32)
            fb = T(ps, nc, "f_b", [H, 1], F32)
            S.dma("sp", sel[:], D["sel"], writes=[kb])
            S.dma("sp", id8[:], D["id8"], writes=[kb])
            S.dma("sp", fg[:], D[f"foxg_{l}"], writes=[kb])
            S.dma("sp", fb[:], D[f"foxb_{l}"], writes=[kb])
            gqs = T(ps, nc, "f_gqs", [128, 1], F32)
            S.op("dve", lambda e: e.tensor_scalar(out=gqs[:], in0=fg[:, 0:1], scalar1=128.0 ** -0.5, scalar2=None, op0=ALU.mult), reads=[kb], writes=[kb])
            nb = T(ps, nc, "f_nb", [H, 1], F32)
            S.op("dve", lambda e: e.tensor_scalar(out=nb[:], in0=fb[:], scalar1=-1.0, scalar2=None, op0=ALU.mult), reads=[kb], writes=[kb])
            pc = [T(ps, nc, f"f_pc{i}", [H, c.S], BF16) for i in range(1)]; pcb = Buf("f_pc")
            negb = T(ps, nc, "f_negb", [128, NJ, H], F32); negbb = Buf("f_negb")
            p_tf = PS(ps, nc, "f_pt", [128, max(c.QR, NJ * H)]); p_tb = Buf("f_pt")
            p_t = p_tf
            cst = contextlib.ExitStack()
            f32 = T(cst, nc, "f_f32", [H, c.S], F32); fbuf = Buf("f_f32")
            cum = T(cst, nc, "f_cum", [H, c.S], F32); cumb = Buf("f_cum")
            one8 = T(cst, nc, "f_one", [H, c.S], F32); oneb = Buf("f_one")
            S.dma("sp", f32[:], D["pf"], reads=[pfb], writes=[fbuf])
            S.op("pool", lambda e: e.memset(one8[:], 1.0), writes=[oneb])
            S.op("act", lambda e: e.activation(out=f32[:], in_=f32[:], func=AF.Exp, bias=nb[:, 0:1], scale=-1.0), reads=[fbuf, kb], writes=[fbuf])
            S.op("act", lambda e: e.activation(out=f32[:], in_=f32[:], func=AF.Ln, bias=1.0, scale=1.0), reads=[fbuf], writes=[fbuf])
            S.op("dve", lambda e: e.tensor_tensor_scan(out=cum[:], data0=one8[:], data1=f32[:], initial=0.0, op0=ALU.mult, op1=ALU.subtract),
                 reads=[oneb, fbuf], writes=[cumb])
            S.op("dve", lambda e: e.tensor_copy(out=pc[0][:], in_=cum[:]), reads=[cumb], writes=[pcb])
            for J in range(NJ):
                S.op("pe", lambda e: e.transpose(p_t[:, J * H:(J + 1) * H], cum[:, J * 128:(J + 1) * 128], id8[:]),
                     reads=[cumb, kb], writes=[p_tb], inc=(J == NJ - 1))
            S.op("dve", lambda e: e.tensor_scalar(out=negb[:, :, :].rearrange("p a b -> p (a b)"), in0=p_t[:, :NJ * H], scalar1=-1.0, scalar2=-FOX_C, op0=ALU.mult, op1=ALU.add),
                 reads=[p_tb], writes=[negbb])
            S.barrier()
            cst.close()
            q32 = T(ps, nc, "f_q32", [128, c.S], F32); q32b = Buf("f_q32")
            k32 = T(ps, nc, "f_k32", [128, c.S], F32); k32b = Buf("f_k32")
            v16 = T(ps, nc, "f_v16", [128, c.S], BF16); v16b = Buf("f_v16")
            sq = T(ps, nc, "f_sq", [128, c.S], F32); sqb = Buf("f_sq")
            rs = T(ps, nc, "f_rs", [128, c.S], F32); rsb = Buf("f_rs")
            qn = [T(ps, nc, f"f_qn{i}", [128, c.S], BF16) for i in range(2)]; qnb = [Buf(f"f_qn{i}") for i in range(2)]
            kn = [T(ps, nc, f"f_kn{i}", [128, c.S], BF16) for i in range(2)]; knb = [Buf(f"f_kn{i}") for i in range(2)]
            vT = [T(ps, nc, f"f_vT{i}", [128, NJ, 128], BF16) for i in range(2)]; vTb = [Buf(f"f_vT{i}") for i in range(2)]
            B16 = [T(ps, nc, f"f_B{i}", [128, c.S], BF16) for i in range(2)]; Bb = [Buf(f"f_B{i}") for i in range(2)]
            NE = 2 if self.filler is not None else 3
            NPV = 1 if self.filler is not None else 2
            e16 = [T(ps, nc, f"f_e{i}", [128, c.QR], BF16) for i in range(NE)]; e16b = [Buf(f"f_e{i}") for i in range(NE)]
            rden = T(ps, nc, "f_rden", [128, c.QR], F32); rdb = Buf("f_rden")
            p_s = [PS(ps, nc, f"f_ps{i}", [128, c.QR]) for i in range(NE)]; p_sb = [Buf(f"f_ps{i}") for i in range(NE)]
            p_n = [PS(ps, nc, f"f_pn{i}", [128, c.QR]) for i in range(1)]; p_nb = [Buf(f"f_pn{i}") for i in range(1)]
            p_d = PS(ps, nc, "f_pd", [128, c.QR]); p_db = Buf("f_pd")
            p_v = [PS(ps, nc, f"f_pv{i}", [128, 128], BF16) for i in range(NPV)]; p_vb = [Buf(f"f_pv{i}") for i in range(NPV)]
            p_ss, p_ssb = p_t, p_tb
            base0 = c.RHL * 8

            def setup_ops(h):
                hp = h % 2
                base = base0 + h * 3
                ops = []
                ops.append(lambda: S.dma("sp", q32[:], D["pz"][base], reads=[pzb], writes=[q32b]))
                ops.append(lambda: S.dma("sp", k32[:], D["pz"][base + 1], reads=[pzb], writes=[k32b]))
                ops.append(lambda: S.dma("pool", v16[:], D["pz"][base + 2], reads=[pzb], writes=[v16b]))
                for (src, srcb, dst, dstb, gcol) in ((q32, q32b, qn[hp], qnb[hp], gqs[:, 0:1]), (k32, k32b, kn[hp], knb[hp], fg[:, 1:2])):
                    def mk(src=src, srcb=srcb, dst=dst, dstb=dstb, gcol=gcol):
                        o = []
                        o.append(lambda: S.op("act", lambda e: e.activation(out=sq[:], in_=src[:], func=AF.Square), reads=[srcb], writes=[sqb]))
                        for r in range(NR):
                            sl = slice(r * c.QR, (r + 1) * c.QR)
                            o.append(lambda sl=sl: S.op("pe", lambda e: e.matmul(p_ss[:, :c.QR], lhsT=self.ones32[:], rhs=sq[:, sl], start=True, stop=True),
                                                        reads=[sqb, self.cb], writes=[p_ssb], inc=True))
                            o.append(lambda sl=sl: S.op("dve", lambda e: e.tensor_scalar(out=rs[:, sl], in0=p_ss[:, :c.QR], scalar1=1.0 / 128, scalar2=EPS,
                                                                                       op0=ALU.mult, op1=ALU.add), reads=[p_ssb], writes=[rsb]))
                        o.append(lambda: S.op("act", lambda e: e.activation(out=rs[:], in_=rs[:], func=AF.Sqrt), reads=[rsb], writes=[rsb]))
                        o.append(lambda: S.op("dve", lambda e: e.reciprocal(out=rs[:], in_=rs[:]), reads=[rsb], writes=[rsb]))
                        o.append(lambda: S.op("dve", lambda e: e.scalar_tensor_tensor(out=dst[:], in0=src[:], scalar=gcol, in1=rs[:], op0=ALU.mult, op1=ALU.mult),
                                              reads=[srcb, rsb, kb], writes=[dstb]))
                        return o
                    ops += mk()
                for J in range(NJ):
                    jp = J % NPV
                    ops.append(lambda J=J, jp=jp: S.op("pe", lambda e: e.transpose(p_v[jp][:], v16[:, J * 128:(J + 1) * 128], self.ident),
                                                       reads=[v16b, self.c16b], writes=[p_vb[jp]], inc=True))
                    ops.append(lambda J=J, jp=jp: S.op("act", lambda e: e.copy(out=vT[hp][:, J, :], in_=p_v[jp][:]), reads=[p_vb[jp]], writes=[vTb[hp]]))
                return ops

            blocks = []
            for r in range(NR):
                for J in range((r + 1) * JR):
                    blocks.append((r, J))
            gblk = [0]

            def scores(h, bi):
                hp = h % 2
                r, J = blocks[bi]
                r0, r1_ = r * c.QR, (r + 1) * c.QR
                i0 = max(r0, J * 128)
                N = r1_ - i0
                b = (gblk[0] + bi) % NE
                S.op("pe", lambda e: e.matmul(p_s[b][:, :N], lhsT=kn[hp][:, J * 128:(J + 1) * 128], rhs=qn[hp][:, i0:r1_], start=True, stop=False),
                     reads=[knb[hp], qnb[hp]], writes=[p_sb[b]], inc=False)
                S.op("pe", lambda e: e.matmul(p_s[b][:, :N], lhsT=sel[:, h * 128:(h + 1) * 128], rhs=pc[0][:, i0:r1_], start=False, stop=True),
                     reads=[pcb, kb], writes=[p_sb[b]], inc=True)
                S.op("act", lambda e: e.activation(out=e16[b][:, :N], in_=p_s[b][:, :N], func=AF.Exp, bias=negb[:, J, h:h + 1], scale=1.0),
                     reads=[p_sb[b], negbb], writes=[e16b[b]])
                if J * 128 >= r0:
                    S.op("pool", lambda e: e.tensor_tensor(out=e16[b][:, 0:128], in0=e16[b][:, 0:128], in1=self.tri, op=ALU.mult),
                         reads=[e16b[b], self.c16b], writes=[e16b[b]])

            def pv(h, bi):
                hp = h % 2
                r, J = blocks[bi]
                r0, r1_ = r * c.QR, (r + 1) * c.QR
                nJ = (r + 1) * JR
                i0 = max(r0, J * 128)
                N = r1_ - i0
                off = i0 - r0
                b = (gblk[0] + bi) % NE
                rp = 0
                S.op("pe", lambda e: e.matmul(p_d[:, off:], lhsT=self.ones16, rhs=e16[b][:, :N], start=(J == 0), stop=(J == nJ - 1)),
                     reads=[self.c16b, e16b[b]], writes=[p_db], inc=False)
                S.op("pe", lambda e: e.matmul(p_n[rp][:, off:], lhsT=vT[hp][:, J, :], rhs=e16[b][:, :N], start=(J == 0), stop=(J == nJ - 1)),
                     reads=[vTb[hp], e16b[b]], writes=[p_nb[rp]], inc=True)
                if J == nJ - 1:
                    S.op("dve", lambda e: e.reciprocal(out=rden[:], in_=p_d[:]), reads=[p_db], writes=[rdb])
                    S.op("dve", lambda e: e.tensor_tensor(out=B16[hp][:, r0:r1_], in0=p_n[rp][:], in1=rden[:], op=ALU.mult),
                         reads=[p_nb[rp], rdb], writes=[Bb[hp]])

            nxt = setup_ops(0)
            for o in nxt:
                o()
            nb_ = len(blocks)
            LOOK = NE - 1
            for h in range(H):
                nxt = setup_ops(h + 1) if h + 1 < H else []
                per = (len(nxt) + nb_ - 1) // nb_ + 1
                for bi in range(min(LOOK, nb_)):
                    scores(h, bi)
                for bi in range(nb_):
                    if bi + LOOK < nb_:
                        scores(h, bi + LOOK)
                    pv(h, bi)
                    self.fill_hook(1)
                    for _ in range(per):
                        if nxt:
                            nxt.pop(0)()
                while nxt:
                    nxt.pop(0)()
                gblk[0] += nb_
                S.dma("sp", self.abcout_ap(1, h), B16[h % 2][:], reads=[Bb[h % 2]], writes=[abcb])
            S.barrier()

    def lru(self, l, gps, pzb, abcb):
        c, S, nc, D = self.cfg, self.S, self.nc, self.Dr
        NR = c.S // c.QR
        L = c.LBL
        with contextlib.ExitStack() as ps:
            kb = Buf("l_consts")
            lp = T(ps, nc, "l_p", [128, L * 8], F32)
            S.dma("sp", lp[:], D[f"lrup_{l}"], writes=[kb])
            sc = T(ps, nc, "l_sc", [128, L], F32)
            for b in range(L):
                S.op("act", lambda e: e.activation(out=sc[:, b:b + 1], in_=lp[:, b * 8 + 7:b * 8 + 8], func=AF.Exp, scale=-1.0), reads=[kb], writes=[kb])
            S.op("act", lambda e: e.activation(out=sc[:], in_=sc[:], func=AF.Ln, bias=1.0, scale=1.0), reads=[kb], writes=[kb])
            S.op("dve", lambda e: e.tensor_scalar(out=sc[:], in0=sc[:], scalar1=-LRU_C, scalar2=None, op0=ALU.mult), reads=[kb], writes=[kb])
            wa = T(ps, nc, "l_wa", [128, 2 * L, 128], BF16); wab = Buf("l_wa")
            for b in range(2 * L):
                S.dma("pool", wa[:, b, :], D[f"lruw_{l}"][b], writes=[wab])
            lx = T(ps, nc, "l_lx", [128, c.S], F32); lxb = Buf("l_lx")
            lg = T(ps, nc, "l_lg", [128, c.S], F32); lgb = Buf("l_lg")
            xc = T(ps, nc, "l_xc", [128, c.S], F32); xcb = Buf("l_xc")
            xc16 = T(ps, nc, "l_xc16", [128, c.S], BF16); xc16b = Buf("l_xc16")
            rr = T(ps, nc, "l_r", [128, c.S], F32); rrb = Buf("l_r")
            ig = T(ps, nc, "l_i", [128, c.S], F32); igb = Buf("l_i")
            aa, aab = rr, rrb
            uu, uub = ig, igb
            hh = T(ps, nc, "l_h", [128, c.S], F32); hhb = Buf("l_h")
            tt, ttb = lx, lxb
            C16 = T(ps, nc, "l_C", [128, c.S], BF16); Cb = Buf("l_C")
            p_r = [PS(ps, nc, f"l_pr{i}", [128, c.QR]) for i in range(2)]; p_rb = [Buf(f"l_pr{i}") for i in range(2)]
            p_i = [PS(ps, nc, f"l_pi{i}", [128, c.QR]) for i in range(2)]; p_ib = [Buf(f"l_pi{i}") for i in range(2)]
            base0 = c.RHL * 8 + c.FHL * 3
            Sn = c.S

            def SF(*a_, **k_):
                r_ = S.op(*a_, **k_)
                self.fill_hook(2)
                return r_
            for b in range(L):
                col = lambda k: lp[:, b * 8 + k:b * 8 + k + 1]
                S.dma("sp", lg[:], D["pz"][base0 + 2 * b], reads=[pzb], writes=[lgb])
                S.dma("sp", lx[:], D["pz"][base0 + 2 * b + 1], reads=[pzb], writes=[lxb])
                SF("dve", lambda e: e.tensor_scalar(out=xc[:], in0=lx[:], scalar1=col(3), scalar2=col(4), op0=ALU.mult, op1=ALU.add),
                     reads=[lxb, kb], writes=[xcb])
                for sh in (1, 2, 3):
                    SF("dve", lambda e: e.scalar_tensor_tensor(out=xc[:, sh:], in0=lx[:, :Sn - sh], scalar=col(3 - sh), in1=xc[:, sh:],
                                                                 op0=ALU.mult, op1=ALU.add), reads=[lxb, xcb, kb], writes=[xcb])
                SF("pool", lambda e: e.tensor_copy(out=xc16[:], in_=xc[:]), reads=[xcb], writes=[xc16b])
                for r in range(NR):
                    sl = slice(r * c.QR, (r + 1) * c.QR)
                    a = r % 2
                    SF("pe", lambda e: e.matmul(p_r[a][:], lhsT=wa[:, b, :], rhs=xc16[:, sl], start=True, stop=True), reads=[wab, xc16b], writes=[p_rb[a]], inc=True)
                    SF("pe", lambda e: e.matmul(p_i[a][:], lhsT=wa[:, L + b, :], rhs=xc16[:, sl], start=True, stop=True), reads=[wab, xc16b], writes=[p_ib[a]], inc=True)
                    SF("act", lambda e: e.activation(out=rr[:, sl], in_=p_r[a][:], func=AF.Sigmoid, bias=col(5), scale=1.0), reads=[p_rb[a], kb], writes=[rrb])
                    SF("act", lambda e: e.activation(out=ig[:, sl], in_=p_i[a][:], func=AF.Sigmoid, bias=col(6), scale=1.0), reads=[p_ib[a], kb], writes=[igb])
                SF("act", lambda e: e.activation(out=aa[:], in_=rr[:], func=AF.Exp, scale=sc[:, b:b + 1]), reads=[rrb, kb], writes=[aab])
                SF("pool", lambda e: e.tensor_tensor(out=tt[:], in0=aa[:], in1=aa[:], op=ALU.mult), reads=[aab], writes=[ttb])
                SF("pool", lambda e: e.tensor_scalar(out=tt[:], in0=tt[:], scalar1=-1.0, scalar2=1.0, op0=ALU.mult, op1=ALU.add), reads=[ttb], writes=[ttb])
                SF("act", lambda e: e.activation(out=tt[:], in_=tt[:], func=AF.Sqrt), reads=[ttb], writes=[ttb])
                SF("dve", lambda e: e.tensor_tensor(out=uu[:], in0=ig[:], in1=xc[:], op=ALU.mult), reads=[igb, xcb], writes=[uub])
                SF("dve", lambda e: e.tensor_tensor(out=uu[:], in0=uu[:], in1=tt[:], op=ALU.mult), reads=[uub, ttb], writes=[uub])
                SF("dve", lambda e: e.tensor_tensor_scan(out=hh[:], data0=aa[:], data1=uu[:], initial=0.0, op0=ALU.mult, op1=ALU.add),
                     reads=[aab, uub], writes=[hhb])
                SF("pool", lambda e: e.tensor_tensor(out=tt[:], in0=lg[:], in1=lg[:], op=ALU.mult), reads=[lgb], writes=[ttb])
                SF("pool", lambda e: e.tensor_scalar(out=tt[:], in0=tt[:], scalar1=0.044715, scalar2=1.0, op0=ALU.mult, op1=ALU.add), reads=[ttb], writes=[ttb])
                SF("pool", lambda e: e.tensor_tensor(out=tt[:], in0=tt[:], in1=lg[:], op=ALU.mult), reads=[ttb, lgb], writes=[ttb])
                SF("act", lambda e: e.activation(out=tt[:], in_=tt[:], func=AF.Sigmoid, scale=2.0 * math.sqrt(2.0 / math.pi)), reads=[ttb], writes=[ttb])
                SF("dve", lambda e: e.tensor_tensor(out=tt[:], in0=tt[:], in1=lg[:], op=ALU.mult), reads=[ttb, lgb], writes=[ttb])
                SF("dve", lambda e: e.tensor_tensor(out=C16[:], in0=tt[:], in1=hh[:], op=ALU.mult), reads=[ttb, hhb], writes=[Cb])
                S.dma("sp", self.abcout_ap(2, b), C16[:], reads=[Cb], writes=[abcb])
            S.barrier()

    def phase3(self, l):
        c, S, nc, D = self.cfg, self.S, self.nc, self.Dr
        gscb = Buf("gsc")
        with contextlib.ExitStack() as ps:
          if not getattr(self, "gates_done", False):
                h16 = T(ps, nc, "p3_h", [128, c.DC, c.TPC], BF16); hb = Buf("p3_h")
                for i in range(c.DC):
                    S.dma("sp", h16[:, i, :], self.hx_ap(i), writes=[hb])
                g16 = [T(ps, nc, f"p3_g{i}", [128, c.TPC], BF16) for i in range(2)]; g16b = [Buf(f"p3_g{i}") for i in range(2)]
                pp = [[PS(ps, nc, f"p3_pp{a}_{t}", [128, c.NB]) for t in range(c.NTB)] for a in range(2)]
                ppb = [[Buf(f"p3pp{a}_{t}") for t in range(c.NTB)] for a in range(2)]
                for cc in range(3 * c.DC):
                    a = cc % 2
                    w, wbf = self.wload(D[f"wmrg_{l}"][cc], c.DC * 128)
                    for t in range(c.NTB):
                        sl = slice(t * c.NB, (t + 1) * c.NB)
                        for kc in range(c.DC):
                            S.op("pe", lambda e: e.matmul(pp[a][t][:], lhsT=w[:, kc * 128:(kc + 1) * 128], rhs=h16[:, kc, sl],
                                                           start=(kc == 0), stop=(kc == c.DC - 1)),
                                 reads=[wbf, hb], writes=[ppb[a][t]], inc=(kc == c.DC - 1))
                        S.op("act", lambda e: e.activation(out=g16[a][:, sl], in_=pp[a][t][:], func=AF.Sigmoid), reads=[ppb[a][t]], writes=[g16b[a]])
                    S.dma("sp", self.gsc_ap(cc), g16[a][:], reads=[g16b[a]], writes=[gscb])
                S.barrier()
        with contextlib.ExitStack() as ps:
            y16 = T(ps, nc, "p3_y", [128, c.DC, c.TPC], BF16); yb = Buf("p3_y")
            with contextlib.ExitStack() as ps2:
                ab = T(ps2, nc, "p3_ab", [128, c.BC, c.TPC], BF16); abb = Buf("p3_ab")
                gt = [T(ps2, nc, f"p3_gt{i}", [128, c.TPC], BF16) for i in range(2)]; gtb = [Buf(f"p3_gt{i}") for i in range(2)]
                tmp = T(ps2, nc, "p3_tmp", [128, c.NB], F32); tmpb = Buf("p3_tmp")
                pp = [[PS(ps2, nc, f"p3_pm{a}_{t}", [128, c.NB]) for t in range(c.NTB)] for a in range(2)]
                ppb = [[Buf(f"p3pm{a}_{t}") for t in range(c.NTB)] for a in range(2)]
                k = 0
                for br in range(3):
                    for kk in range(c.BC):
                        S.dma("sp", ab[:, kk, :], self.abcin_ap(br, kk), writes=[abb])
                    for i in range(c.DC):
                        a = k % 2
                        k += 1
                        w, wbf = self.wload(D[f"wbr_{l}"][br * c.DC + i], c.BC * 128)
                        S.dma("sp", gt[a][:], self.gsc_ap(br * c.DC + i), reads=[gscb], writes=[gtb[a]])
                        for t in range(c.NTB):
                            sl = slice(t * c.NB, (t + 1) * c.NB)
                            for kk in range(c.BC):
                                S.op("pe", lambda e: e.matmul(pp[a][t][:], lhsT=w[:, kk * 128:(kk + 1) * 128], rhs=ab[:, kk, sl],
                                                               start=(kk == 0), stop=(kk == c.BC - 1)),
                                     reads=[wbf, abb], writes=[ppb[a][t]], inc=(kk == c.BC - 1))
                            if br == 0:
                                S.op("dve", lambda e: e.tensor_tensor(out=y16[:, i, sl], in0=pp[a][t][:], in1=gt[a][:, sl], op=ALU.mult),
                                     reads=[ppb[a][t], gtb[a]], writes=[yb])
                            else:
                                S.op("dve", lambda e: e.tensor_tensor(out=tmp[:], in0=pp[a][t][:], in1=gt[a][:, sl], op=ALU.mult),
                                     reads=[ppb[a][t], gtb[a]], writes=[tmpb])
                                S.op("dve", lambda e: e.tensor_tensor(out=y16[:, i, sl], in0=y16[:, i, sl], in1=tmp[:], op=ALU.add),
                                     reads=[tmpb, yb], writes=[yb])
                S.barrier()
            with contextlib.ExitStack() as ps2:
                pp = [[PS(ps2, nc, f"p3_po{a}_{t}", [128, c.NB]) for t in range(c.NTB)] for a in range(2)]
                ppb = [[Buf(f"p3po{a}_{t}") for t in range(c.NTB)] for a in range(2)]
                for i in range(c.DC):
                    a = i % 2
                    w, wbf = self.wload(D[f"wout_{l}"][i], c.DC * 128)
                    xt, xb = self.xslot()
                    S.dma("sp", xt[:], self.xs_ap(i), reads=[self.xsb[i]], writes=[xb])
                    for t in range(c.NTB):
                        sl = slice(t * c.NB, (t + 1) * c.NB)
                        for kc in range(c.DC):
                            S.op("pe", lambda e: e.matmul(pp[a][t][:], lhsT=w[:, kc * 128:(kc + 1) * 128], rhs=y16[:, kc, sl],
                                                           start=(kc == 0), stop=(kc == c.DC - 1)),
                                 reads=[wbf, yb], writes=[ppb[a][t]], inc=(kc == c.DC - 1))
                        S.op("dve", lambda e: e.tensor_tensor(out=xt[:, sl], in0=pp[a][t][:], in1=xt[:, sl], op=ALU.add),
                             reads=[ppb[a][t], xb], writes=[xb])
                    S.dma("sp", self.xs_ap(i), xt[:], reads=[xb], writes=[self.xsb[i]])
                S.barrier()


def pretile(W):
    K, M = W.shape
    return np.ascontiguousarray(W.reshape(K // 128, 128, M // 128, 128).transpose(2, 1, 0, 3)).reshape(M // 128, 128, K)


def vec_fm(v):
    return np.ascontiguousarray(v.reshape(-1, 128).T)


def consts(cfg, g):
    c = cfg
    out = {}
    half = 128
    inv_freq = ROPE_BASE ** (-np.arange(half, dtype=np.float32) / half)
    ang = np.arange(c.S, dtype=np.float32)[None, :] * inv_freq[:, None]
    out["cs"] = np.stack([np.cos(ang), np.sin(ang)]).astype(np.float32)
    heads = np.arange(g * c.RHL, (g + 1) * c.RHL)
    lg = np.log(1.0 - np.exp2(-5.0 - heads.astype(np.float32))).astype(np.float32)
    idx = np.arange(128, dtype=np.float32)
    rel = idx[None, :] - idx[:, None]
    sc = np.float32(256.0 ** -0.5)
    rmask = np.where(rel[None] >= 0, np.exp(np.maximum(rel, 0)[None] * lg[:, None, None]), 0.0) * sc
    out["rmask"] = rmask.astype(np.float32)
    rvec = np.zeros((128, c.RHL * 3), np.float32)
    for h in range(c.RHL):
        rvec[:, 3 * h] = np.exp((127.0 - idx) * lg[h]) * sc
        rvec[:, 3 * h + 1] = np.exp((idx + 1.0) * lg[h])
        rvec[:, 3 * h + 2] = np.exp(128.0 * lg[h])
    out["rvec"] = rvec
    tri = (idx[None, :] >= idx[:, None]).astype(np.float32)
    cb = np.concatenate([tri, np.eye(128, dtype=np.float32), np.ones((128, 128), np.float32)], axis=1)
    out["cb16"] = cb.astype(NPBF)
    sel = np.zeros((c.FHL, c.FHL, 128), np.float32)
    for h in range(c.FHL):
        sel[h, h, :] = 1.0
    out["sel"] = sel.reshape(c.FHL, c.FHL * 128).astype(NPBF)
    out["id8"] = np.eye(c.FHL, dtype=np.float32)
    return out


def mix_cols(cfg, g):
    c = cfg
    o_rq, o_rk, o_rv, o_rg = 0, c.RW, 2 * c.RW, 3 * c.RW
    o_fq = 4 * c.RW
    o_fk, o_fv = o_fq + c.FW, o_fq + 2 * c.FW
    o_fg = o_fq + 3 * c.FW
    o_lg = o_fg + c.FH
    o_lx = o_lg + c.LW
    o_m = o_lx + c.LW
    cols = []
    for hl in range(c.RHL):
        h = g * c.RHL + hl
        for o in (o_rq, o_rk, o_rv, o_rg):
            cols.append(np.arange(o + h * 256, o + (h + 1) * 256))
    for hl in range(c.FHL):
        h = g * c.FHL + hl
        for o in (o_fq, o_fk, o_fv):
            cols.append(np.arange(o + h * 128, o + (h + 1) * 128))
    for bl in range(c.LBL):
        b = g * c.LBL + bl
        for o in (o_lg, o_lx):
            cols.append(np.arange(o + b * 128, o + (b + 1) * 128))
    cols = np.concatenate(cols)
    fcols = np.arange(o_fg + g * c.FHL, o_fg + (g + 1) * c.FHL)
    mcols = np.arange(o_m, o_m + 3 * c.D)
    return cols, fcols, mcols


def ffn_inputs(cfg, p, inp, name, l):
    c = cfg
    d = {}
    d[p + "_n"] = vec_fm(inp[f"{name}_norm"][l])
    d[p + "_wg"] = pretile(inp[f"{name}_w_gate"][l])
    d[p + "_wu"] = pretile(inp[f"{name}_w_up"][l])
    Wd = inp[f"{name}_w_down"][l]
    d[p + "_wd"] = np.ascontiguousarray(
        Wd.reshape(c.FQ, c.AG, 128, c.DC, 128).transpose(0, 3, 2, 1, 4)).reshape(c.FQ * c.DC, 128, c.AG * 128)
    return d


_CACHE = {}


def run_chain(cfg, inp, dbg=None):
    c = cfg
    ncores = 2 * c.B
    x = inp["x"]
    ones32 = np.ones((128, 128), np.float32)
    xs = []
    for core in range(ncores):
        b, g = core // 2, core % 2
        xt = x[b, g * c.TPC:(g + 1) * c.TPC, :]
        xs.append(np.ascontiguousarray(xt.T).reshape(c.DC, 128, c.TPC))
    K = [consts(c, g) for g in range(2)]
    for l in range(c.DEPTH):
        xs, hx = launch_tok(c, "p1", l, inp, xs, None, None, ones32)
        if dbg is not None:
            dbg[f"x1_{l}"] = xs
            dbg[f"hx_{l}"] = hx
        prog = get_prog(c, (("p2", 0),))
        maps = []
        for core in range(ncores):
            b, g = core // 2, core % 2
            cols, fcols, mcols = mix_cols(c, g)
            W = inp["w_in"][l]
            m = {"hfull": np.stack([hx[2 * b], hx[2 * b + 1]]), "ones32": ones32}
            m["wmix_0"] = pretile(np.ascontiguousarray(W[:, cols]))
            m["wfg_0"] = np.ascontiguousarray(W[:, fcols].reshape(c.DC, 128, c.FHL).transpose(1, 0, 2)).reshape(128, c.DC * c.FHL)
            rn = inp["ret_norm"][l][g * c.RHL:(g + 1) * c.RHL]
            m["retgn_0"] = np.ascontiguousarray(rn.reshape(c.RHL * 2, 128).T)
            m["foxg_0"] = np.ascontiguousarray(np.stack([inp["fox_q_norm"][l], inp["fox_k_norm"][l]], axis=1))
            m["foxb_0"] = np.ascontiguousarray(inp["fox_f_bias"][l][g * c.FHL:(g + 1) * c.FHL].reshape(c.FHL, 1))
            lp = np.zeros((128, c.LBL, 8), np.float32)
            for bl in range(c.LBL):
                sl = slice((g * c.LBL + bl) * 128, (g * c.LBL + bl + 1) * 128)
                lp[:, bl, 0:4] = inp["lru_conv_w"][l][:, sl].T
                lp[:, bl, 4] = inp["lru_conv_b"][l][sl]
                lp[:, bl, 5] = inp["lru_b_a"][l][sl]
                lp[:, bl, 6] = inp["lru_b_x"][l][sl]
                lp[:, bl, 7] = inp["lru_lambda"][l][sl]
            m["lrup_0"] = lp.reshape(128, c.LBL * 8)
            m["lruw_0"] = np.ascontiguousarray(np.concatenate([inp["lru_w_a"][l][g * c.LBL:(g + 1) * c.LBL],
                                                               inp["lru_w_x"][l][g * c.LBL:(g + 1) * c.LBL]]))
            m.update(K[g])
            maps.append(m)
        res = run_bass_kernel_spmd(prog.nc, maps, core_ids=list(range(ncores)))
        abc = [r["abc_out"] for r in res.results]
        if dbg is not None:
            dbg[f"abc_{l}"] = abc
        abc_in = []
        for core in range(ncores):
            b, g = core // 2, core % 2
            tsl = slice(g * c.TPC, (g + 1) * c.TPC)
            parts = [abc[2 * b + gg][:, :, :, tsl] for gg in range(2)]
            abc_in.append(np.ascontiguousarray(np.concatenate(parts, axis=1)))
        xs, _ = launch_tok(c, "p3", l, inp, xs, hx, abc_in, ones32)
        if dbg is not None:
            dbg[f"x3_{l}"] = xs
    out = np.empty((c.B, c.S, c.D), np.float32)
    for core in range(ncores):
        b, g = core // 2, core % 2
        out[b, g * c.TPC:(g + 1) * c.TPC, :] = xs[core].reshape(c.D, c.TPC).T
    return out


def get_prog(cfg, segs):
    key = (id(cfg), segs)
    if key not in _CACHE:
        p = Prog(cfg, list(segs), None)
        p.build()
        _CACHE[key] = p
    return _CACHE[key]


def launch_tok(c, kind, l, inp, xs, hx, abc_in, ones32):
    prog = get_prog(c, ((kind, 0),))
    ncores = 2 * c.B
    shared = {"ones32": ones32}
    if kind == "p1":
        shared.update(ffn_inputs(c, "f1_0", inp, "ffn1", l))
        shared["n_mix_0"] = vec_fm(inp["mix_norm"][l])
    else:
        _, _, mcols = mix_cols(c, 0)
        shared["wmrg_0"] = pretile(np.ascontiguousarray(inp["w_in"][l][:, mcols]))
        shared["wbr_0"] = np.concatenate([pretile(inp[n][l]) for n in ("w_branch_ret", "w_branch_fox", "w_branch_lru")])
        shared["wout_0"] = pretile(inp["w_out"][l])
        shared.update(ffn_inputs(c, "f2_0", inp, "ffn2", l))
    maps = []
    for core in range(ncores):
        m = dict(shared)
        m["x_in"] = xs[core]
        if kind == "p3":
            m["abc_in"] = abc_in[core]
            m["hx"] = hx[core]
        maps.append(m)
    res = run_bass_kernel_spmd(prog.nc, maps, core_ids=list(range(ncores)))
    xs2 = [r["xs"] for r in res.results]
    hx2 = [r["hx"] for r in res.results] if kind == "p1" else hx
    return xs2, hx2


def run_fused(cfg, inp):
    c = cfg
    key = ("full", id(cfg))
    if key not in _CACHE:
        p = Prog(cfg, [], None)
        p.build_full()
        _CACHE[key] = p
    prog = _CACHE[key]
    x = inp["x"]
    shared = {"ones32": np.ones((128, 128), np.float32)}
    shared.update(consts(c, 0))
    cols, fcols, mcols = mix_cols(c, 0)
    for l in range(c.DEPTH):
        shared.update(ffn_inputs(c, f"f1_{l}", inp, "ffn1", l))
        shared.update(ffn_inputs(c, f"f2_{l}", inp, "ffn2", l))
        shared[f"n_mix_{l}"] = vec_fm(inp["mix_norm"][l])
        W = inp["w_in"][l]
        shared[f"wmix_{l}"] = pretile(np.ascontiguousarray(W[:, cols]))
        shared[f"wfg_{l}"] = np.ascontiguousarray(W[:, fcols].reshape(c.DC, 128, c.FHL).transpose(1, 0, 2)).reshape(128, c.DC * c.FHL)
        shared[f"retgn_{l}"] = np.ascontiguousarray(inp["ret_norm"][l].reshape(c.RHL * 2, 128).T)
        shared[f"foxg_{l}"] = np.ascontiguousarray(np.stack([inp["fox_q_norm"][l], inp["fox_k_norm"][l]], axis=1))
        shared[f"foxb_{l}"] = np.ascontiguousarray(inp["fox_f_bias"][l].reshape(c.FHL, 1))
        lp = np.zeros((128, c.LBL, 8), np.float32)
        for bl in range(c.LBL):
            sl = slice(bl * 128, (bl + 1) * 128)
            lp[:, bl, 0:4] = inp["lru_conv_w"][l][:, sl].T
            lp[:, bl, 4] = inp["lru_conv_b"][l][sl]
            lp[:, bl, 5] = inp["lru_b_a"][l][sl]
            lp[:, bl, 6] = inp["lru_b_x"][l][sl]
            lp[:, bl, 7] = inp["lru_lambda"][l][sl]
        shared[f"lrup_{l}"] = lp.reshape(128, c.LBL * 8)
        shared[f"lruw_{l}"] = np.ascontiguousarray(np.concatenate([inp["lru_w_a"][l], inp["lru_w_x"][l]]))
        shared[f"wmrg_{l}"] = pretile(np.ascontiguousarray(W[:, mcols]))
        shared[f"wbr_{l}"] = np.concatenate([pretile(inp[n][l]) for n in ("w_branch_ret", "w_branch_fox", "w_branch_lru")])
        shared[f"wout_{l}"] = pretile(inp["w_out"][l])
    maps = []
    for b in range(c.B):
        m = dict(shared)
        m["x_in"] = np.ascontiguousarray(x[b].T).reshape(c.DC, 128, 2, c.TPC).transpose(2, 0, 1, 3).copy()
        maps.append(m)
    res = run_bass_kernel_spmd(prog.nc, maps, core_ids=list(range(c.B)))
    out = np.empty((c.B, c.S, c.D), np.float32)
    for b in range(c.B):
        xs = res.results[b]["xs"]
        out[b] = xs.transpose(0, 3, 1, 2).reshape(c.S, c.D)
    return out


def kernel(**inputs):
    cfg = _CACHE.setdefault("cfg", CFG(full=True))
    inp = {k: np.asarray(v) for k, v in inputs.items()}
    return run_fused(cfg, inp)
```

```python
import contextlib
import math
import os
import numpy as np
import ml_dtypes
import concourse.bass as bass
import concourse.mybir as mybir
from concourse.bass_utils import run_bass_kernel_spmd

F32 = mybir.dt.float32
BF16 = mybir.dt.bfloat16
ALU = mybir.AluOpType
AF = mybir.ActivationFunctionType
NPBF = ml_dtypes.bfloat16

SEM_ROT = 30000
EPS = 1e-6
ROPE_BASE = 10000.0
LRU_C = 8.0
FOX_C = 16.0


class CFG:
    def __init__(self, D=4096, S=2048, B=4, DEPTH=2, NB=512, QR=512, AG=16, full=False):
        self.D, self.S, self.B, self.DEPTH = D, S, B, DEPTH
        self.full = full
        self.FF = 2 * D
        self.RW = self.FW = self.LW = D // 2
        self.RH = self.RW // 256
        self.FH = self.FW // 128
        self.LB = self.LW // 128
        dv = 1 if full else 2
        self.RHL, self.FHL, self.LBL = self.RH // dv, self.FH // dv, self.LB // dv
        self.TPC = S // 2
        self.NB = min(NB, self.TPC)
        self.NTB = self.TPC // self.NB
        self.QR = min(QR, S)
        self.DC = D // 128
        self.FFC = self.FF // 128
        self.AG = min(AG, self.FFC)
        self.FQ = self.FFC // self.AG
        self.BC = self.RW // 128
        self.BCL = self.BC // dv
        self.NCC = self.RHL * 8 + self.FHL * 3 + self.LBL * 2
        self.N_IN = 4 * self.RW + 3 * self.FW + self.FH + 2 * self.LW + 3 * D
        self.WSZ = max(self.DC, self.AG, self.BC) * 128


class _Buf:
    __slots__ = ("name", "w", "r", "sem", "semv")

    def __init__(self, name):
        self.name = name
        self.w = {}
        self.r = {}
        self.sem = None
        self.semv = 0


_BUFS = {}


def Buf(name):
    b = _BUFS.get(name)
    if b is None:
        b = _BUFS[name] = _Buf(name)
    return b


class Sched:
    def __init__(self, nc, stack):
        _BUFS.clear()
        self.nc = nc
        self.stack = stack
        self.engs = {"pe": nc.tensor, "act": nc.scalar, "dve": nc.vector, "pool": nc.gpsimd, "sp": nc.sync}
        self.esem = {}
        self.seen = {e: {} for e in self.engs}
        self.nsem = 0
        self.nwait = 0
        self.nins = {e: 0 for e in self.engs}
        self.dbufs = []
        self.allsems = []
        self.freesems = {}
        self.semq = {}
        self.persist = set()
        for e in self.engs:
            self.esem[e] = [self.new_sem(e), 0]

    def new_sem(self, tag):
        self.nsem += 1
        s = self.stack.enter_context(self.nc.semaphore(f"s{self.nsem}_{tag}"))
        return s

    def _deps(self, reads, writes):
        d = {}
        for b in reads:
            for k, sv in b.w.items():
                if k not in d or d[k][1] < sv[1]:
                    d[k] = sv
        for b in writes:
            for dd in (b.w, b.r):
                for k, sv in dd.items():
                    if k not in d or d[k][1] < sv[1]:
                        d[k] = sv
        return d

    def _wait(self, e, deps):
        seen = self.seen[e]
        own = id(self.esem[e][0])
        for k, (s, v) in deps.items():
            if e == "pe" and k == own:
                continue
            if seen.get(k, 0) >= v:
                continue
            self.engs[e].wait_ge(s, v)
            self.nwait += 1
            seen[k] = v

    def _record(self, rec, reads, writes):
        k = id(rec[0])
        for b in reads:
            b.r[k] = rec
        for b in writes:
            b.w[k] = rec

    def op(self, e, fn, reads=(), writes=(), inc=True):
        self._wait(e, self._deps(reads, writes))
        ins = fn(self.engs[e])
        self.nins[e] += 1
        st = self.esem[e]
        if inc:
            st[1] += 1
            ins.then_inc(st[0], 1)
            rec = (st[0], st[1])
            if st[1] >= SEM_ROT:
                self.allsems.append((st[0], st[1]))
                self.esem[e] = [self.new_sem(e), 0]
        else:
            rec = (st[0], st[1] + 1)
        self._record(rec, reads, writes)
        return ins

    def dma(self, q, out, in_, reads=(), writes=(), sem_buf=None):
        sb = sem_buf if sem_buf is not None else (writes[0] if writes else reads[0])
        if sb.sem is None:
            fl = self.freesems.get(q)
            if fl:
                sb.sem, sb.semv = fl.pop()
            else:
                sb.sem = self.new_sem("d" + sb.name)
                self.semq[id(sb.sem)] = q
            self.dbufs.append(sb)
        assert self.semq[id(sb.sem)] == q, (sb.name, q)
        self._wait(q, self._deps(reads, writes))
        ins = self.engs[q].dma_start(out=out, in_=in_)
        self.nins[q] += 1
        sb.semv += 16
        ins.then_inc(sb.sem, 16)
        rec = (sb.sem, sb.semv)
        self._record(rec, reads, writes)
        return ins

    def barrier(self, engines=("pe", "act", "dve", "pool", "sp")):
        d = {}
        for e, (s, v) in self.esem.items():
            if v > 0:
                d[id(s)] = (s, v)
        for s, v in self.allsems:
            d[id(s)] = (s, v)
        for b in self.dbufs:
            if b.semv > 0:
                d[id(b.sem)] = (b.sem, b.semv)
        for e in engines:
            self._wait(e, d)
        keep = []
        for b in self.dbufs:
            if True:
                keep.append(b)
            else:
                self.allsems.append((b.sem, b.semv))
                self.freesems.setdefault(self.semq[id(b.sem)], []).append((b.sem, b.semv))
                b.sem = None
        self.dbufs = keep


_UID = [0]


def T(pool_stack, nc, name, shape, dt):
    _UID[0] += 1
    return pool_stack.enter_context(nc.sbuf_tensor(f"{name}_u{_UID[0]}", list(shape), dt))


def PS(pool_stack, nc, name, shape, dt=F32):
    _UID[0] += 1
    return pool_stack.enter_context(nc.psum_tensor(f"{name}_u{_UID[0]}", list(shape), dt))


class GateFiller:
    def __init__(self, P, l, ps):
        c, nc = P.cfg, P.nc
        self.P, self.l = P, l
        self.h = T(ps, nc, "gf_h", [128, c.DC, c.TPC], BF16)
        self.hb = Buf("gf_h")
        self.g = [T(ps, nc, f"gf_g{i}", [128, c.NB], BF16) for i in range(2)]
        self.gb = [Buf(f"gf_g{i}") for i in range(2)]
        self.pf = [PS(ps, nc, f"gf_p{i}", [128, c.NB]) for i in range(2)]
        self.pfb = [Buf(f"gf_p{i}") for i in range(2)]
        self.gscb = Buf("gsc")
        self.gen = self._gen()

    def _gen(self):
        P, l = self.P, self.l
        c, S, D = P.cfg, P.S, P.Dr
        chunks = [(th, cc) for th in range(2) for cc in range(3 * c.DC)]
        pend = {}

        def issue(idx):
            cc = chunks[idx][1]
            pend[idx] = P.wload(D[f"wmrg_{l}"][cc], c.DC * 128)

        for i in range(min(2, len(chunks))):
            issue(i)
        k = 0
        for idx, (th, cc) in enumerate(chunks):
            if cc == 0:
                for i in range(c.DC):
                    S.dma("sp", self.h[:, i, :], D["hx"][th, i], writes=[self.hb])
            if idx + 2 < len(chunks):
                issue(idx + 2)
            w, wbf = pend.pop(idx)
            for tq in range(c.NTB):
                sl = slice(tq * c.NB, (tq + 1) * c.NB)
                a = k % 2
                k += 1
                for kc in range(c.DC):
                    S.op("pe", lambda e: e.matmul(self.pf[a][:], lhsT=w[:, kc * 128:(kc + 1) * 128], rhs=self.h[:, kc, sl],
                                                   start=(kc == 0), stop=(kc == c.DC - 1)),
                         reads=[wbf, self.hb], writes=[self.pfb[a]], inc=(kc == c.DC - 1))
                    if kc % 8 == 7 and kc != c.DC - 1:
                        yield
                S.op("act", lambda e: e.activation(out=self.g[a][:], in_=self.pf[a][:], func=AF.Sigmoid),
                     reads=[self.pfb[a]], writes=[self.gb[a]])
                S.dma("sp", D["gsc"][th, cc][:, sl], self.g[a][:], reads=[self.gb[a]], writes=[self.gscb])
                yield

    def fill(self, n):
        for _ in range(n):
            if next(self.gen, "done") == "done":
                return

    def drain(self):
        for _ in self.gen:
            pass

class Prog:
    def __init__(self, cfg, segments, layer_ids):
        self.cfg = c = cfg
        self.nc = nc = bass.Bass("TRN2", target_bir_lowering=False)
        self.segments = segments
        self.ext_in = {}
        self.ext_out = {}
        kinds = [k for k, _ in segments]
        self.kinds = kinds

    def din(self, name, shape, dt=F32):
        t = self.nc.dram_tensor(name, list(shape), dt, kind="ExternalInput").ap()
        self.ext_in[name] = (tuple(shape), dt)
        return t

    def dout(self, name, shape, dt=F32):
        t = self.nc.dram_tensor(name, list(shape), dt, kind="ExternalOutput").ap()
        self.ext_out[name] = (tuple(shape), dt)
        return t

    def dscr(self, name, shape, dt=F32):
        return self.nc.dram_tensor(name, list(shape), dt, kind="Internal").ap()

    def build(self):
        c, nc = self.cfg, self.nc
        kinds = self.kinds
        first, last = kinds[0], kinds[-1]
        has_tok = ("p1" in kinds) or ("p3" in kinds)
        with contextlib.ExitStack() as gs:
            self.gs = gs
            self.S = S = Sched(nc, gs)
            D = {}
            self.Dr = D
            if has_tok:
                D["x_in"] = self.din("x_in", [c.DC, 128, c.TPC])
                D["xs"] = self.dout("xs", [c.DC, 128, c.TPC])
            if "p1" in kinds:
                D["hx"] = self.dout("hx", [c.DC, 128, c.TPC], BF16)
            elif "p3" in kinds:
                D["hx"] = self.din("hx", [c.DC, 128, c.TPC], BF16)
            if "p2" in kinds:
                D["hfull"] = self.din("hfull", [2, c.DC, 128, c.TPC], BF16)
                D["abc_out"] = self.dout("abc_out", [3, c.BCL, 128, c.S], BF16)
                D["pz"] = self.dscr("pz", [c.NCC, 128, c.S])
                D["pf"] = self.dscr("pf", [c.FHL, c.S])
            if "p3" in kinds:
                D["abc_in"] = self.din("abc_in", [3, c.BC, 128, c.TPC], BF16)
                D["gsc"] = self.dscr("gsc", [3 * c.DC, 128, c.TPC], BF16)
            for kind, l in self.segments:
                if kind == "p1":
                    self._decl_ffn(D, f"f1_{l}")
                    D[f"n_mix_{l}"] = self.din(f"n_mix_{l}", [128, c.DC])
                elif kind == "p2":
                    D[f"wmix_{l}"] = self.din(f"wmix_{l}", [c.NCC, 128, c.DC * 128])
                    D[f"wfg_{l}"] = self.din(f"wfg_{l}", [128, c.DC * c.FHL])
                    D[f"retgn_{l}"] = self.din(f"retgn_{l}", [128, c.RHL * 2])
                    D[f"foxg_{l}"] = self.din(f"foxg_{l}", [128, 2])
                    D[f"foxb_{l}"] = self.din(f"foxb_{l}", [c.FHL, 1])
                    D[f"lrup_{l}"] = self.din(f"lrup_{l}", [128, c.LBL * 8])
                    D[f"lruw_{l}"] = self.din(f"lruw_{l}", [2 * c.LBL, 128, 128])
                elif kind == "p3":
                    D[f"wmrg_{l}"] = self.din(f"wmrg_{l}", [3 * c.DC, 128, c.DC * 128])
                    D[f"wbr_{l}"] = self.din(f"wbr_{l}", [3 * c.DC, 128, c.BC * 128])
                    D[f"wout_{l}"] = self.din(f"wout_{l}", [c.DC, 128, c.DC * 128])
                    self._decl_ffn(D, f"f2_{l}")
            if "p2" in kinds:
                D["cs"] = self.din("cs", [2, 128, c.S])
                D["rmask"] = self.din("rmask", [c.RHL, 128, 128])
                D["rvec"] = self.din("rvec", [128, c.RHL * 3])
                D["cb16"] = self.din("cb16", [128, 3 * 128], BF16)
                D["sel"] = self.din("sel", [c.FHL, c.FHL * 128], BF16)
                D["id8"] = self.din("id8", [c.FHL, c.FHL])
            D["ones32"] = self.din("ones32", [128, 128])

            self.NW = 5
            self.wsl = [T(gs, nc, f"wsl{i}", [128, c.WSZ], BF16) for i in range(self.NW)]
            self.wb = [Buf(f"w{i}") for i in range(self.NW)]
            self.wi = 0
            self.xt = [T(gs, nc, f"xt{i}", [128, c.TPC], F32) for i in range(3)]
            self.xtb = [Buf(f"xt{i}") for i in range(3)]
            self.xi = 0
            self.ones32 = T(gs, nc, "ones32", [128, 128], F32)
            self.cb = Buf("consts")
            S.dma("sp", self.ones32[:], D["ones32"], writes=[self.cb])
            self.xsb = [Buf(f"xs{i}") for i in range(c.DC)] if has_tok else []

            if has_tok:
                for i in range(c.DC):
                    S.dma("sp", D["xs"][i], D["x_in"][i], writes=[self.xsb[i]])
            for kind, l in self.segments:
                if kind == "p1":
                    self.ffn(f"f1_{l}")
                    self.mixnorm(l)
                elif kind == "p2":
                    self.phase2(l)
                elif kind == "p3":
                    self.phase3(l)
                    self.ffn(f"f2_{l}")
            S.barrier()
            print("prog", self.segments, "nins", S.nins, "nwait", S.nwait, "nsem", S.nsem)
        return nc


    def xs_ap(self, i):
        return self.Dr["xs"][self.th, i] if self.cfg.full else self.Dr["xs"][i]

    def hx_ap(self, i):
        return self.Dr["hx"][self.th, i] if self.cfg.full else self.Dr["hx"][i]

    def hfull_ap(self, th, i):
        return self.Dr["hx"][th, i] if self.cfg.full else self.Dr["hfull"][th, i]

    def abcin_ap(self, br, kk):
        c = self.cfg
        if c.full:
            return self.Dr["abc"][br, kk][:, self.th * c.TPC:(self.th + 1) * c.TPC]
        return self.Dr["abc_in"][br, kk]

    def abcout_ap(self, br, ch):
        return self.Dr["abc"][br, ch] if self.cfg.full else self.Dr["abc_out"][br, ch]

    def build_full(self):
        c, nc = self.cfg, self.nc
        with contextlib.ExitStack() as gs:
            self.gs = gs
            self.S = S = Sched(nc, gs)
            D = {}
            self.Dr = D
            D["x_in"] = self.din("x_in", [2, c.DC, 128, c.TPC])
            D["xs"] = self.dout("xs", [2, c.DC, 128, c.TPC])
            D["hx"] = self.dscr("hx", [2, c.DC, 128, c.TPC], BF16)
            D["abc"] = self.dscr("abc", [3, c.BC, 128, c.S], BF16)
            D["pz"] = self.dscr("pz", [c.NCC, 128, c.S])
            D["pf"] = self.dscr("pf", [c.FHL, c.S])
            D["gsc"] = self.dscr("gsc", [2, 3 * c.DC, 128, c.TPC], BF16)
            for l in range(c.DEPTH):
                self._decl_ffn(D, f"f1_{l}")
                D[f"n_mix_{l}"] = self.din(f"n_mix_{l}", [128, c.DC])
                D[f"wmix_{l}"] = self.din(f"wmix_{l}", [c.NCC, 128, c.DC * 128])
                D[f"wfg_{l}"] = self.din(f"wfg_{l}", [128, c.DC * c.FHL])
                D[f"retgn_{l}"] = self.din(f"retgn_{l}", [128, c.RHL * 2])
                D[f"foxg_{l}"] = self.din(f"foxg_{l}", [128, 2])
                D[f"foxb_{l}"] = self.din(f"foxb_{l}", [c.FHL, 1])
                D[f"lrup_{l}"] = self.din(f"lrup_{l}", [128, c.LBL * 8])
                D[f"lruw_{l}"] = self.din(f"lruw_{l}", [2 * c.LBL, 128, 128])
                D[f"wmrg_{l}"] = self.din(f"wmrg_{l}", [3 * c.DC, 128, c.DC * 128])
                D[f"wbr_{l}"] = self.din(f"wbr_{l}", [3 * c.DC, 128, c.BC * 128])
                D[f"wout_{l}"] = self.din(f"wout_{l}", [c.DC, 128, c.DC * 128])
                self._decl_ffn(D, f"f2_{l}")
            D["cs"] = self.din("cs", [2, 128, c.S])
            D["rmask"] = self.din("rmask", [c.RHL, 128, 128])
            D["rvec"] = self.din("rvec", [128, c.RHL * 3])
            D["cb16"] = self.din("cb16", [128, 3 * 128], BF16)
            D["sel"] = self.din("sel", [c.FHL, c.FHL * 128], BF16)
            D["id8"] = self.din("id8", [c.FHL, c.FHL])
            D["ones32"] = self.din("ones32", [128, 128])
            self.NW = 5
            self.wsl = [T(gs, nc, f"wsl{i}", [128, c.WSZ], BF16) for i in range(self.NW)]
            self.wb = [Buf(f"w{i}") for i in range(self.NW)]
            self.wi = 0
            self.xt = [T(gs, nc, f"xt{i}", [128, c.TPC], F32) for i in range(3)]
            self.xtb = [Buf(f"xt{i}") for i in range(3)]
            self.xi = 0
            self.ones32 = T(gs, nc, "ones32", [128, 128], F32)
            self.cb = Buf("consts")
            S.dma("sp", self.ones32[:], D["ones32"], writes=[self.cb])
            self.xsb = [Buf(f"xs{i}") for i in range(c.DC)]
            for th in range(2):
                for i in range(c.DC):
                    S.dma("sp", D["xs"][th, i], D["x_in"][th, i], writes=[self.xsb[i]])
            if os.environ.get("B0", "1") == "1":
                S.barrier()
            nst = int(os.environ.get("FUSE_STAGES", "99"))
            st = 0
            for l in range(c.DEPTH):
                for th in range(2):
                    self.th = th
                    if st < nst:
                        self.ffn(f"f1_{l}")
                    st += 1
                    if st < nst:
                        self.mixnorm(l)
                    st += 1
                if st < nst:
                    self.phase2(l)
                st += 1
                for th in range(2):
                    self.th = th
                    if st < nst:
                        self.phase3(l)
                    st += 1
                    if st < nst:
                        self.ffn(f"f2_{l}")
                    st += 1
            S.barrier()
            print("prog full nins", S.nins, "nwait", S.nwait, "nsem", S.nsem)
        return nc

    filler = None

    def fill_hook(self, n):
        if self.filler is not None:
            self.filler.fill(n)

    def gsc_ap(self, idx):
        return self.Dr["gsc"][self.th, idx] if self.cfg.full else self.Dr["gsc"][idx]

    def _decl_ffn(self, D, p):
        c = self.cfg
        D[p + "_n"] = self.din(p + "_n", [128, c.DC])
        D[p + "_wg"] = self.din(p + "_wg", [c.FFC, 128, c.DC * 128])
        D[p + "_wu"] = self.din(p + "_wu", [c.FFC, 128, c.DC * 128])
        D[p + "_wd"] = self.din(p + "_wd", [c.FQ * c.DC, 128, c.AG * 128])

    def wload(self, src, width):
        i = self.wi
        self.wi = (i + 1) % self.NW
        self.S.dma("pool", self.wsl[i][:, :width], src, writes=[self.wb[i]])
        return self.wsl[i], self.wb[i]

    def xslot(self):
        i = self.xi
        self.xi = (i + 1) % 3
        return self.xt[i], self.xtb[i]

    def rmsnorm_fm(self, ps, gname, out16, outb):
        c, S, nc, D = self.cfg, self.S, self.nc, self.Dr
        gt = T(ps, nc, "nrm_g", [128, c.DC], F32)
        gb = Buf("nrm_g")
        S.dma("sp", gt[:], D[gname], writes=[gb])
        sq = [T(ps, nc, f"nrm_sq{i}", [128, c.TPC], F32) for i in range(2)]
        sqb = [Buf(f"nrm_sq{i}") for i in range(2)]
        rstd = T(ps, nc, "nrm_rstd", [128, c.TPC], F32)
        rb = Buf("nrm_rstd")
        pss = [PS(ps, nc, f"nrm_ps{t}", [128, c.NB]) for t in range(c.NTB)]
        pssb = [Buf(f"nrm_ps{t}") for t in range(c.NTB)]
        for i in range(c.DC):
            xt, xb = self.xslot()
            S.dma("sp", xt[:], self.xs_ap(i), reads=[self.xsb[i]], writes=[xb])
            s = i % 2
            S.op("act", lambda e: e.activation(out=sq[s][:], in_=xt[:], func=AF.Square), reads=[xb], writes=[sqb[s]])
            S.op("dve", lambda e: e.tensor_scalar(out=out16[:, i, :], in0=xt[:], scalar1=gt[:, i:i + 1], scalar2=None, op0=ALU.mult),
                 reads=[xb, gb], writes=[outb])
            for t in range(c.NTB):
                S.op("pe", lambda e: e.matmul(pss[t][:], lhsT=self.ones32[:], rhs=sq[s][:, t * c.NB:(t + 1) * c.NB],
                                               start=(i == 0), stop=(i == c.DC - 1)),
                     reads=[sqb[s], self.cb], writes=[pssb[t]], inc=True)
        for t in range(c.NTB):
            sl = slice(t * c.NB, (t + 1) * c.NB)
            S.op("dve", lambda e: e.tensor_scalar(out=rstd[:, sl], in0=pss[t][:], scalar1=1.0 / c.D, scalar2=EPS,
                                                  op0=ALU.mult, op1=ALU.add), reads=[pssb[t]], writes=[rb])
        S.op("act", lambda e: e.activation(out=rstd[:], in_=rstd[:], func=AF.Sqrt), reads=[rb], writes=[rb])
        S.op("dve", lambda e: e.reciprocal(out=rstd[:], in_=rstd[:]), reads=[rb], writes=[rb])
        for i in range(c.DC):
            eng = "dve" if i % 2 == 0 else "pool"
            S.op(eng, lambda e: e.tensor_tensor(out=out16[:, i, :], in0=out16[:, i, :], in1=rstd[:], op=ALU.mult),
                 reads=[rb, outb], writes=[outb])

    def ffn(self, p):
        c, S, nc, D = self.cfg, self.S, self.nc, self.Dr
        with contextlib.ExitStack() as ps:
            h16 = T(ps, nc, "ffn_h", [128, c.DC, c.TPC], BF16)
            hb = Buf("ffn_h")
            with contextlib.ExitStack() as ps2:
                self.rmsnorm_fm(ps2, p + "_n", h16, hb)
                S.barrier()
            act = T(ps, nc, "ffn_act", [128, c.AG, c.TPC], BF16)
            actb = Buf("ffn_act")
            sg = [T(ps, nc, f"ffn_sg{i}", [128, c.NB], F32) for i in range(2)]
            sgb = [Buf(f"ffn_sg{i}") for i in range(2)]
            npb = 2 if 4 * c.NTB <= 8 else 1
            pg = [[PS(ps, nc, f"ffn_pg{a}_{t}", [128, c.NB]) for t in range(c.NTB)] for a in range(npb)]
            pu = [[PS(ps, nc, f"ffn_pu{a}_{t}", [128, c.NB]) for t in range(c.NTB)] for a in range(npb)]
            pgb = [[Buf(f"pg{a}_{t}") for t in range(c.NTB)] for a in range(npb)]
            pub = [[Buf(f"pu{a}_{t}") for t in range(c.NTB)] for a in range(npb)]
            k = 0
            for q in range(c.FQ):
                for jj in range(c.AG):
                    j = q * c.AG + jj
                    a = j % npb
                    wg, wgb = self.wload(D[p + "_wg"][j], c.DC * 128)
                    wu, wub = self.wload(D[p + "_wu"][j], c.DC * 128)
                    for t in range(c.NTB):
                        sl = slice(t * c.NB, (t + 1) * c.NB)
                        for kc in range(c.DC):
                            S.op("pe", lambda e: e.matmul(pg[a][t][:], lhsT=wg[:, kc * 128:(kc + 1) * 128], rhs=h16[:, kc, sl],
                                                           start=(kc == 0), stop=(kc == c.DC - 1)),
                                 reads=[wgb, hb], writes=[pgb[a][t]], inc=(kc == c.DC - 1))
                        for kc in range(c.DC):
                            S.op("pe", lambda e: e.matmul(pu[a][t][:], lhsT=wu[:, kc * 128:(kc + 1) * 128], rhs=h16[:, kc, sl],
                                                           start=(kc == 0), stop=(kc == c.DC - 1)),
                                 reads=[wub, hb], writes=[pub[a][t]], inc=(kc == c.DC - 1))
                        s = k % 2
                        k += 1
                        S.op("act", lambda e: e.activation(out=sg[s][:], in_=pg[a][t][:], func=AF.Silu),
                             reads=[pgb[a][t]], writes=[sgb[s]])
                        S.op("dve", lambda e: e.tensor_tensor(out=act[:, jj, sl], in0=sg[s][:], in1=pu[a][t][:], op=ALU.mult),
                             reads=[sgb[s], pub[a][t]], writes=[actb])
                for i in range(c.DC):
                    a = i % npb
                    wd, wdb = self.wload(D[p + "_wd"][q * c.DC + i], c.AG * 128)
                    xt, xb = self.xslot()
                    S.dma("sp", xt[:], self.xs_ap(i), reads=[self.xsb[i]], writes=[xb])
                    for t in range(c.NTB):
                        sl = slice(t * c.NB, (t + 1) * c.NB)
                        for kk in range(c.AG):
                            S.op("pe", lambda e: e.matmul(pg[a][t][:], lhsT=wd[:, kk * 128:(kk + 1) * 128], rhs=act[:, kk, sl],
                                                           start=(kk == 0), stop=(kk == c.AG - 1)),
                                 reads=[wdb, actb], writes=[pgb[a][t]], inc=(kk == c.AG - 1))
                        S.op("dve", lambda e: e.scalar_tensor_tensor(out=xt[:, sl], in0=pg[a][t][:], scalar=0.5, in1=xt[:, sl],
                                                                     op0=ALU.mult, op1=ALU.add),
                             reads=[pgb[a][t], xb], writes=[xb])
                    S.dma("sp", self.xs_ap(i), xt[:], reads=[xb], writes=[self.xsb[i]])
            S.barrier()

    def mixnorm(self, l):
        c, S, nc, D = self.cfg, self.S, self.nc, self.Dr
        with contextlib.ExitStack() as ps:
            h16 = T(ps, nc, "mn_h", [128, c.DC, c.TPC], BF16)
            hb = Buf("mn_h")
            self.rmsnorm_fm(ps, f"n_mix_{l}", h16, hb)
            hxb = Buf("hx")
            for i in range(c.DC):
                S.dma("sp", self.hx_ap(i), h16[:, i, :], reads=[hb], writes=[hxb])
            S.barrier()

    def phase2(self, l):
        c, S, nc, D = self.cfg, self.S, self.nc, self.Dr
        pzb = Buf("pz")
        pfb = Buf("pf")
        with contextlib.ExitStack() as ps:
            h16 = T(ps, nc, "p2_h", [128, c.DC, c.TPC], BF16)
            hb = Buf("p2_h")
            wfg = T(ps, nc, "p2_wfg", [128, c.DC * c.FHL], BF16)
            wfgb = Buf("p2_wfg")
            S.dma("pool", wfg[:], D[f"wfg_{l}"], writes=[wfgb])
            st = [T(ps, nc, f"p2_st{i}", [128, c.TPC], F32) for i in range(2)]
            stb = [Buf(f"p2_st{i}") for i in range(2)]
            stf = T(ps, nc, "p2_stf", [c.FHL, c.TPC], F32)
            stfb = Buf("p2_stf")
            pp = [[PS(ps, nc, f"p2_pp{a}_{t}", [128, c.NB]) for t in range(c.NTB)] for a in range(2)]
            ppb = [[Buf(f"p2pp{a}_{t}") for t in range(c.NTB)] for a in range(2)]
            for th in range(2):
                for i in range(c.DC):
                    S.dma("sp", h16[:, i, :], self.hfull_ap(th, i), writes=[hb])
                for cc in range(c.NCC):
                    a = cc % 2
                    w, wbf = self.wload(D[f"wmix_{l}"][cc], c.DC * 128)
                    for t in range(c.NTB):
                        sl = slice(t * c.NB, (t + 1) * c.NB)
                        for kc in range(c.DC):
                            S.op("pe", lambda e: e.matmul(pp[a][t][:], lhsT=w[:, kc * 128:(kc + 1) * 128], rhs=h16[:, kc, sl],
                                                           start=(kc == 0), stop=(kc == c.DC - 1)),
                                 reads=[wbf, hb], writes=[ppb[a][t]], inc=(kc == c.DC - 1))
                        S.op("act", lambda e: e.copy(out=st[a][:, sl], in_=pp[a][t][:]), reads=[ppb[a][t]], writes=[stb[a]])
                    S.dma("sp", D["pz"][cc, :, th * c.TPC:(th + 1) * c.TPC], st[a][:], reads=[stb[a]], writes=[pzb])
                for t in range(c.NTB):
                    sl = slice(t * c.NB, (t + 1) * c.NB)
                    for kc in range(c.DC):
                        S.op("pe", lambda e: e.matmul(pp[0][t][:c.FHL, :], lhsT=wfg[:, kc * c.FHL:(kc + 1) * c.FHL], rhs=h16[:, kc, sl],
                                                       start=(kc == 0), stop=(kc == c.DC - 1)),
                             reads=[wfgb, hb], writes=[ppb[0][t]], inc=(kc == c.DC - 1))
                    S.op("act", lambda e: e.copy(out=stf[:, sl], in_=pp[0][t][:c.FHL, :]), reads=[ppb[0][t]], writes=[stfb])
                S.dma("sp", D["pf"][:, th * c.TPC:(th + 1) * c.TPC], stf[:], reads=[stfb], writes=[pfb])
            S.barrier()
        abcb = Buf("abc_out")
        with contextlib.ExitStack() as ps:
            cb16 = T(ps, nc, "cb16", [128, 3 * 128], BF16)
            kb = Buf("cb16")
            S.dma("sp", cb16[:], D["cb16"], writes=[kb])
            self.tri, self.ident, self.ones16, self.c16b = cb16[:, 0:128], cb16[:, 128:256], cb16[:, 256:384], kb
            skip = os.environ.get("SKIPMIX", "")
            if "r" not in skip:
                self.retention(l, ps, pzb, abcb)
            with contextlib.ExitStack() as fs:
                if c.full and os.environ.get("NOFILL", "0") != "1":
                    self.filler = GateFiller(self, l, fs)
                if "f" not in skip:
                    self.fox(l, ps, pzb, pfb, abcb)
                if "l" not in skip:
                    self.lru(l, ps, pzb, abcb)
                if self.filler is not None:
                    self.filler.drain()
                    S.barrier()
                    self.filler = None
                    self.gates_done = True

    def retention(self, l, gps, pzb, abcb):
        c, S, nc, D = self.cfg, self.S, self.nc, self.Dr
        NCH = c.S // 128
        with contextlib.ExitStack() as ps:
            cs = T(ps, nc, "r_cs", [128, 2, c.S], F32)
            rmask = T(ps, nc, "r_mask", [128, c.RHL, 128], F32)
            rvec = T(ps, nc, "r_vec", [128, c.RHL * 3], F32)
            gn = T(ps, nc, "r_gn", [128, c.RHL * 2], F32)
            kb = Buf("r_consts")
            S.dma("sp", cs[:, 0, :], D["cs"][0], writes=[kb])
            S.dma("sp", cs[:, 1, :], D["cs"][1], writes=[kb])
            for h in range(c.RHL):
                S.dma("sp", rmask[:, h, :], D["rmask"][h], writes=[kb])
            S.dma("sp", rvec[:], D["rvec"], writes=[kb])
            S.dma("sp", gn[:], D[f"retgn_{l}"], writes=[kb])
            q32 = T(ps, nc, "r_q32", [128, 2, c.S], F32); q32b = Buf("r_q32")
            k32 = T(ps, nc, "r_k32", [128, 2, c.S], F32); k32b = Buf("r_k32")
            v16 = T(ps, nc, "r_v16", [128, 2, c.S], BF16); v16b = Buf("r_v16")
            qr = T(ps, nc, "r_qr", [128, 2, c.S], BF16); qrb = Buf("r_qr")
            kr = T(ps, nc, "r_kr", [128, 2, c.S], BF16); krb = Buf("r_kr")
            sg16 = T(ps, nc, "r_sg", [128, 2, c.S], BF16); sgb = Buf("r_sg")
            A16 = T(ps, nc, "r_A", [128, 2, c.S], BF16); Ab = Buf("r_A")
            t1 = T(ps, nc, "r_t1", [128, c.S], F32); t1b = Buf("r_t1")
            t2 = T(ps, nc, "r_t2", [128, c.S], F32); t2b = Buf("r_t2")
            st32 = T(ps, nc, "r_st32", [128, 2, 256], F32); st32b = Buf("r_st32")
            st16 = T(ps, nc, "r_st16", [128, 2, 256], BF16); st16b = Buf("r_st16")
            sm16 = T(ps, nc, "r_sm16", [128, 128], BF16); smb = Buf("r_sm16")
            kd16 = T(ps, nc, "r_kd16", [128, 256], BF16); kdb = Buf("r_kd16")
            vT16 = T(ps, nc, "r_vT16", [128, 256], BF16); vTb = Buf("r_vT16")
            in32 = T(ps, nc, "r_in32", [128, 256], F32); inb = Buf("r_in32")
            o32 = T(ps, nc, "r_o32", [128, 256], F32); ob = Buf("r_o32")
            junk = T(ps, nc, "r_junk", [128, 256], F32); jb = Buf("r_junk")
            ss = T(ps, nc, "r_ss", [128, 1], F32); ssb = Buf("r_ss")
            y16 = T(ps, nc, "r_y16", [128, 256], BF16); yb = Buf("r_y16")
            p_s = PS(ps, nc, "r_ps", [128, 128]); p_sb = Buf("r_ps")
            p_kT = PS(ps, nc, "r_pkT", [128, 256], BF16); p_kTb = Buf("r_pkT")
            p_vT = PS(ps, nc, "r_pvT", [128, 256], BF16); p_vTb = Buf("r_pvT")
            p_o = PS(ps, nc, "r_po", [128, 256]); p_ob = Buf("r_po")
            p_c = PS(ps, nc, "r_pc", [128, 256]); p_cb = Buf("r_pc")
            p_kv = PS(ps, nc, "r_pkv", [128, 2, 256]); p_kvb = Buf("r_pkv")
            p_yT = PS(ps, nc, "r_pyT", [128, 2, 128], BF16); p_yTb = Buf("r_pyT")
            cos, sin = cs[:, 0, :], cs[:, 1, :]
            for h in range(c.RHL):
                base = h * 8
                for cc in range(2):
                    S.dma("sp", q32[:, cc, :], D["pz"][base + cc], reads=[pzb], writes=[q32b])
                    S.dma("sp", k32[:, cc, :], D["pz"][base + 2 + cc], reads=[pzb], writes=[k32b])
                    S.dma("pool", v16[:, cc, :], D["pz"][base + 4 + cc], reads=[pzb], writes=[v16b])
                for (src, srcb, dst, dstb) in ((q32, q32b, qr, qrb), (k32, k32b, kr, krb)):
                    S.op("dve", lambda e: e.tensor_tensor(out=t1[:], in0=src[:, 0, :], in1=cos, op=ALU.mult), reads=[srcb, kb], writes=[t1b])
                    S.op("pool", lambda e: e.tensor_tensor(out=t2[:], in0=src[:, 1, :], in1=sin, op=ALU.mult), reads=[srcb, kb], writes=[t2b])
                    S.op("dve", lambda e: e.tensor_tensor(out=dst[:, 0, :], in0=t1[:], in1=t2[:], op=ALU.subtract), reads=[t1b, t2b], writes=[dstb])
                    S.op("dve", lambda e: e.tensor_tensor(out=t1[:], in0=src[:, 1, :], in1=cos, op=ALU.mult), reads=[srcb, kb], writes=[t1b])
                    S.op("pool", lambda e: e.tensor_tensor(out=t2[:], in0=src[:, 0, :], in1=sin, op=ALU.mult), reads=[srcb, kb], writes=[t2b])
                    S.op("dve", lambda e: e.tensor_tensor(out=dst[:, 1, :], in0=t1[:], in1=t2[:], op=ALU.add), reads=[t1b, t2b], writes=[dstb])
                for cc in range(2):
                    S.dma("sp", t1[:], D["pz"][base + 6 + cc], reads=[pzb], writes=[t1b])
                    S.op("act", lambda e: e.activation(out=sg16[:, cc, :], in_=t1[:], func=AF.Silu), reads=[t1b], writes=[sgb])
                kdec, dq, gC = rvec[:, 3 * h:3 * h + 1], rvec[:, 3 * h + 1:3 * h + 2], rvec[:, 3 * h + 2:3 * h + 3]
                for n in range(NCH):
                    tk = slice(n * 128, (n + 1) * 128)
                    for cc in range(2):
                        S.op("pe", lambda e: e.matmul(p_s[:], lhsT=kr[:, cc, tk], rhs=qr[:, cc, tk], start=(cc == 0), stop=(cc == 1)),
                             reads=[krb, qrb], writes=[p_sb], inc=(cc == 1))
                    S.op("dve", lambda e: e.tensor_tensor(out=sm16[:], in0=p_s[:], in1=rmask[:, h, :], op=ALU.mult),
                         reads=[p_sb, kb], writes=[smb])
                    for cc in range(2):
                        S.op("pe", lambda e: e.transpose(p_kT[:, cc * 128:(cc + 1) * 128], kr[:, cc, tk], self.ident),
                             reads=[krb, self.c16b], writes=[p_kTb], inc=(cc == 1))
                    S.op("dve", lambda e: e.tensor_scalar(out=kd16[:], in0=p_kT[:], scalar1=kdec, scalar2=None, op0=ALU.mult),
                         reads=[p_kTb, kb], writes=[kdb])
                    for cc in range(2):
                        S.op("pe", lambda e: e.transpose(p_vT[:, cc * 128:(cc + 1) * 128], v16[:, cc, tk], self.ident),
                             reads=[v16b, self.c16b], writes=[p_vTb], inc=(cc == 1))
                    S.op("act", lambda e: e.copy(out=vT16[:], in_=p_vT[:]), reads=[p_vTb], writes=[vTb])
                    S.op("pe", lambda e: e.matmul(p_o[:], lhsT=sm16[:], rhs=vT16[:], start=True, stop=True),
                         reads=[smb, vTb], writes=[p_ob], inc=True)
                    if n > 0:
                        for cc in range(2):
                            S.op("pe", lambda e: e.matmul(p_c[:], lhsT=qr[:, cc, tk], rhs=st16[:, cc, :], start=(cc == 0), stop=(cc == 1)),
                                 reads=[qrb, st16b], writes=[p_cb], inc=(cc == 1))
                        S.op("act", lambda e: e.copy(out=in32[:], in_=p_o[:]), reads=[p_ob], writes=[inb])
                        S.op("dve", lambda e: e.scalar_tensor_tensor(out=o32[:], in0=p_c[:], scalar=dq, in1=in32[:], op0=ALU.mult, op1=ALU.add),
                             reads=[p_cb, inb, kb], writes=[ob])
                    else:
                        S.op("act", lambda e: e.copy(out=o32[:], in_=p_o[:]), reads=[p_ob], writes=[ob])
                    if n < NCH - 1:
                        for cc in range(2):
                            S.op("pe", lambda e: e.matmul(p_kv[:, cc, :], lhsT=kd16[:, cc * 128:(cc + 1) * 128], rhs=vT16[:], start=True, stop=True),
                                 reads=[kdb, vTb], writes=[p_kvb], inc=(cc == 1))
                        if n == 0:
                            S.op("dve", lambda e: e.tensor_copy(out=st32[:], in_=p_kv[:]), reads=[p_kvb], writes=[st32b])
                        else:
                            for cc in range(2):
                                S.op("dve", lambda e: e.scalar_tensor_tensor(out=st32[:, cc, :], in0=st32[:, cc, :], scalar=gC, in1=p_kv[:, cc, :],
                                                                             op0=ALU.mult, op1=ALU.add),
                                     reads=[p_kvb, st32b, kb], writes=[st32b])
                        S.op("pool", lambda e: e.tensor_copy(out=st16[:], in_=st32[:]), reads=[st32b], writes=[st16b])
                    S.op("act", lambda e: e.activation(out=junk[:], in_=o32[:], func=AF.Square, accum_out=ss[:]), reads=[ob], writes=[jb, ssb])
                    S.op("dve", lambda e: e.tensor_scalar(out=ss[:], in0=ss[:], scalar1=1.0 / 256, scalar2=EPS, op0=ALU.mult, op1=ALU.add),
                         reads=[ssb], writes=[ssb])
                    S.op("act", lambda e: e.activation(out=ss[:], in_=ss[:], func=AF.Sqrt), reads=[ssb], writes=[ssb])
                    S.op("dve", lambda e: e.reciprocal(out=ss[:], in_=ss[:]), reads=[ssb], writes=[ssb])
                    S.op("dve", lambda e: e.tensor_scalar(out=y16[:], in0=o32[:], scalar1=ss[:, 0:1], scalar2=None, op0=ALU.mult),
                         reads=[ob, ssb], writes=[yb])
                    for cc in range(2):
                        S.op("pe", lambda e: e.transpose(p_yT[:, cc, :], y16[:, cc * 128:(cc + 1) * 128], self.ident),
                             reads=[yb, self.c16b], writes=[p_yTb], inc=(cc == 1))
                    for cc in range(2):
                        S.op("dve", lambda e: e.scalar_tensor_tensor(out=A16[:, cc, tk], in0=p_yT[:, cc, :], scalar=gn[:, 2 * h + cc:2 * h + cc + 1],
                                                                     in1=sg16[:, cc, tk], op0=ALU.mult, op1=ALU.mult),
                             reads=[p_yTb, sgb, kb], writes=[Ab])
                for cc in range(2):
                    S.dma("sp", self.abcout_ap(0, h * 2 + cc), A16[:, cc, :], reads=[Ab], writes=[abcb])
            S.barrier()

    def fox(self, l, gps, pzb, pfb, abcb):
        c, S, nc, D = self.cfg, self.S, self.nc, self.Dr
        NJ = c.S // 128
        NR = c.S // c.QR
        JR = c.QR // 128
        H = c.FHL
        with contextlib.ExitStack() as ps:
            kb = Buf("f_consts")
            sel = T(ps, nc, "f_sel", [H, H * 128], BF16)
            id8 = T(ps, nc, "f_id8", [H, H], F32)
            fg = T(ps, nc, "f_g", [128, 2], F32)
            fb = T(ps, nc, "f_b", [H, 1], F32)
            S.dma("sp", sel[:], D["sel"], writes=[kb])
            S.dma("sp", id8[:], D["id8"], writes=[kb])
            S.dma("sp", fg[:], D[f"foxg_{l}"], writes=[kb])
            S.dma("sp", fb[:], D[f"foxb_{l}"], writes=[kb])
            gqs = T(ps, nc, "f_gqs", [128, 1], F32)
            S.op("dve", lambda e: e.tensor_scalar(out=gqs[:], in0=fg[:, 0:1], scalar1=128.0 ** -0.5, scalar2=None, op0=ALU.mult), reads=[kb], writes=[kb])
            nb = T(ps, nc, "f_nb", [H, 1], F32)
            S.op("dve", lambda e: e.tensor_scalar(out=nb[:], in0=fb[:], scalar1=-1.0, scalar2=None, op0=ALU.mult), reads=[kb], writes=[kb])
            pc = [T(ps, nc, f"f_pc{i}", [H, c.S], BF16) for i in range(1)]; pcb = Buf("f_pc")
            negb = T(ps, nc, "f_negb", [128, NJ, H], F32); negbb = Buf("f_negb")
            p_tf = PS(ps, nc, "f_pt", [128, max(c.QR, NJ * H)]); p_tb = Buf("f_pt")
            p_t = p_tf
            cst = contextlib.ExitStack()
            f32 = T(cst, nc, "f_f32", [H, c.S], F32); fbuf = Buf("f_f32")
            cum = T(cst, nc, "f_cum", [H, c.S], F32); cumb = Buf("f_cum")
            one8 = T(cst, nc, "f_one", [H, c.S], F32); oneb = Buf("f_one")
            S.dma("sp", f32[:], D["pf"], reads=[pfb], writes=[fbuf])
            S.op("pool", lambda e: e.memset(one8[:], 1.0), writes=[oneb])
            S.op("act", lambda e: e.activation(out=f32[:], in_=f32[:], func=AF.Exp, bias=nb[:, 0:1], scale=-1.0), reads=[fbuf, kb], writes=[fbuf])
            S.op("act", lambda e: e.activation(out=f32[:], in_=f32[:], func=AF.Ln, bias=1.0, scale=1.0), reads=[fbuf], writes=[fbuf])
            S.op("dve", lambda e: e.tensor_tensor_scan(out=cum[:], data0=one8[:], data1=f32[:], initial=0.0, op0=ALU.mult, op1=ALU.subtract),
                 reads=[oneb, fbuf], writes=[cumb])
            S.op("dve", lambda e: e.tensor_copy(out=pc[0][:], in_=cum[:]), reads=[cumb], writes=[pcb])
            for J in range(NJ):
                S.op("pe", lambda e: e.transpose(p_t[:, J * H:(J + 1) * H], cum[:, J * 128:(J + 1) * 128], id8[:]),
                     reads=[cumb, kb], writes=[p_tb], inc=(J == NJ - 1))
            S.op("dve", lambda e: e.tensor_scalar(out=negb[:, :, :].rearrange("p a b -> p (a b)"), in0=p_t[:, :NJ * H], scalar1=-1.0, scalar2=-FOX_C, op0=ALU.mult, op1=ALU.add),
                 reads=[p_tb], writes=[negbb])
            S.barrier()
            cst.close()
            q32 = T(ps, nc, "f_q32", [128, c.S], F32); q32b = Buf("f_q32")
            k32 = T(ps, nc, "f_k32", [128, c.S], F32); k32b = Buf("f_k32")
            v16 = T(ps, nc, "f_v16", [128, c.S], BF16); v16b = Buf("f_v16")
            sq = T(ps, nc, "f_sq", [128, c.S], F32); sqb = Buf("f_sq")
            rs = T(ps, nc, "f_rs", [128, c.S], F32); rsb = Buf("f_rs")
            qn = [T(ps, nc, f"f_qn{i}", [128, c.S], BF16) for i in range(2)]; qnb = [Buf(f"f_qn{i}") for i in range(2)]
            kn = [T(ps, nc, f"f_kn{i}", [128, c.S], BF16) for i in range(2)]; knb = [Buf(f"f_kn{i}") for i in range(2)]
            vT = [T(ps, nc, f"f_vT{i}", [128, NJ, 128], BF16) for i in range(2)]; vTb = [Buf(f"f_vT{i}") for i in range(2)]
            B16 = [T(ps, nc, f"f_B{i}", [128, c.S], BF16) for i in range(2)]; Bb = [Buf(f"f_B{i}") for i in range(2)]
            NE = 2 if self.filler is not None else 3
            NPV = 1 if self.filler is not None else 2
            e16 = [T(ps, nc, f"f_e{i}", [128, c.QR], BF16) for i in range(NE)]; e16b = [Buf(f"f_e{i}") for i in range(NE)]
            rden = T(ps, nc, "f_rden", [128, c.QR], F32); rdb = Buf("f_rden")
            p_s = [PS(ps, nc, f"f_ps{i}", [128, c.QR]) for i in range(NE)]; p_sb = [Buf(f"f_ps{i}") for i in range(NE)]
            p_n = [PS(ps, nc, f"f_pn{i}", [128, c.QR]) for i in range(1)]; p_nb = [Buf(f"f_pn{i}") for i in range(1)]
            p_d = PS(ps, nc, "f_pd", [128, c.QR]); p_db = Buf("f_pd")
            p_v = [PS(ps, nc, f"f_pv{i}", [128, 128], BF16) for i in range(NPV)]; p_vb = [Buf(f"f_pv{i}") for i in range(NPV)]
            p_ss, p_ssb = p_t, p_tb
            base0 = c.RHL * 8

            def setup_ops(h):
                hp = h % 2
                base = base0 + h * 3
                ops = []
                ops.append(lambda: S.dma("sp", q32[:], D["pz"][base], reads=[pzb], writes=[q32b]))
                ops.append(lambda: S.dma("sp", k32[:], D["pz"][base + 1], reads=[pzb], writes=[k32b]))
                ops.append(lambda: S.dma("pool", v16[:], D["pz"][base + 2], reads=[pzb], writes=[v16b]))
                for (src, srcb, dst, dstb, gcol) in ((q32, q32b, qn[hp], qnb[hp], gqs[:, 0:1]), (k32, k32b, kn[hp], knb[hp], fg[:, 1:2])):
                    def mk(src=src, srcb=srcb, dst=dst, dstb=dstb, gcol=gcol):
                        o = []
                        o.append(lambda: S.op("act", lambda e: e.activation(out=sq[:], in_=src[:], func=AF.Square), reads=[srcb], writes=[sqb]))
                        for r in range(NR):
                            sl = slice(r * c.QR, (r + 1) * c.QR)
                            o.append(lambda sl=sl: S.op("pe", lambda e: e.matmul(p_ss[:, :c.QR], lhsT=self.ones32[:], rhs=sq[:, sl], start=True, stop=True),
                                                        reads=[sqb, self.cb], writes=[p_ssb], inc=True))
                            o.append(lambda sl=sl: S.op("dve", lambda e: e.tensor_scalar(out=rs[:, sl], in0=p_ss[:, :c.QR], scalar1=1.0 / 128, scalar2=EPS,
                                                                                       op0=ALU.mult, op1=ALU.add), reads=[p_ssb], writes=[rsb]))
                        o.append(lambda: S.op("act", lambda e: e.activation(out=rs[:], in_=rs[:], func=AF.Sqrt), reads=[rsb], writes=[rsb]))
                        o.append(lambda: S.op("dve", lambda e: e.reciprocal(out=rs[:], in_=rs[:]), reads=[rsb], writes=[rsb]))
                        o.append(lambda: S.op("dve", lambda e: e.scalar_tensor_tensor(out=dst[:], in0=src[:], scalar=gcol, in1=rs[:], op0=ALU.mult, op1=ALU.mult),
                                              reads=[srcb, rsb, kb], writes=[dstb]))
                        return o
                    ops += mk()
                for J in range(NJ):
                    jp = J % NPV
                    ops.append(lambda J=J, jp=jp: S.op("pe", lambda e: e.transpose(p_v[jp][:], v16[:, J * 128:(J + 1) * 128], self.ident),
                                                       reads=[v16b, self.c16b], writes=[p_vb[jp]], inc=True))
                    ops.append(lambda J=J, jp=jp: S.op("act", lambda e: e.copy(out=vT[hp][:, J, :], in_=p_v[jp][:]), reads=[p_vb[jp]], writes=[vTb[hp]]))
                return ops

            blocks = []
            for r in range(NR):
                for J in range((r + 1) * JR):
                    blocks.append((r, J))
            gblk = [0]

            def scores(h, bi):
                hp = h % 2
                r, J = blocks[bi]
                r0, r1_ = r * c.QR, (r + 1) * c.QR
                i0 = max(r0, J * 128)
                N = r1_ - i0
                b = (gblk[0] + bi) % NE
                S.op("pe", lambda e: e.matmul(p_s[b][:, :N], lhsT=kn[hp][:, J * 128:(J + 1) * 128], rhs=qn[hp][:, i0:r1_], start=True, stop=False),
                     reads=[knb[hp], qnb[hp]], writes=[p_sb[b]], inc=False)
                S.op("pe", lambda e: e.matmul(p_s[b][:, :N], lhsT=sel[:, h * 128:(h + 1) * 128], rhs=pc[0][:, i0:r1_], start=False, stop=True),
                     reads=[pcb, kb], writes=[p_sb[b]], inc=True)
                S.op("act", lambda e: e.activation(out=e16[b][:, :N], in_=p_s[b][:, :N], func=AF.Exp, bias=negb[:, J, h:h + 1], scale=1.0),
                     reads=[p_sb[b], negbb], writes=[e16b[b]])
                if J * 128 >= r0:
                    S.op("pool", lambda e: e.tensor_tensor(out=e16[b][:, 0:128], in0=e16[b][:, 0:128], in1=self.tri, op=ALU.mult),
                         reads=[e16b[b], self.c16b], writes=[e16b[b]])

            def pv(h, bi):
                hp = h % 2
                r, J = blocks[bi]
                r0, r1_ = r * c.QR, (r + 1) * c.QR
                nJ = (r + 1) * JR
                i0 = max(r0, J * 128)
                N = r1_ - i0
                off = i0 - r0
                b = (gblk[0] + bi) % NE
                rp = 0
                S.op("pe", lambda e: e.matmul(p_d[:, off:], lhsT=self.ones16, rhs=e16[b][:, :N], start=(J == 0), stop=(J == nJ - 1)),
                     reads=[self.c16b, e16b[b]], writes=[p_db], inc=False)
                S.op("pe", lambda e: e.matmul(p_n[rp][:, off:], lhsT=vT[hp][:, J, :], rhs=e16[b][:, :N], start=(J == 0), stop=(J == nJ - 1)),
                     reads=[vTb[hp], e16b[b]], writes=[p_nb[rp]], inc=True)
                if J == nJ - 1:
                    S.op("dve", lambda e: e.reciprocal(out=rden[:], in_=p_d[:]), reads=[p_db], writes=[rdb])
                    S.op("dve", lambda e: e.tensor_tensor(out=B16[hp][:, r0:r1_], in0=p_n[rp][:], in1=rden[:], op=ALU.mult),
                         reads=[p_nb[rp], rdb], writes=[Bb[hp]])

            nxt = setup_ops(0)
            for o in nxt:
                o()
            nb_ = len(blocks)
            LOOK = NE - 1
            for h in range(H):
                nxt = setup_ops(h + 1) if h + 1 < H else []
                per = (len(nxt) + nb_ - 1) // nb_ + 1
                for bi in range(min(LOOK, nb_)):
                    scores(h, bi)
                for bi in range(nb_):
                    if bi + LOOK < nb_:
                        scores(h, bi + LOOK)
                    pv(h, bi)
                    self.fill_hook(1)
                    for _ in range(per):
                        if nxt:
                            nxt.pop(0)()
                while nxt:
                    nxt.pop(0)()
                gblk[0] += nb_
                S.dma("sp", self.abcout_ap(1, h), B16[h % 2][:], reads=[Bb[h % 2]], writes=[abcb])
            S.barrier()

    def lru(self, l, gps, pzb, abcb):
        c, S, nc, D = self.cfg, self.S, self.nc, self.Dr
        NR = c.S // c.QR
        L = c.LBL
        with contextlib.ExitStack() as ps:
            kb = Buf("l_consts")
            lp = T(ps, nc, "l_p", [128, L * 8], F32)
            S.dma("sp", lp[:], D[f"lrup_{l}"], writes=[kb])
            sc = T(ps, nc, "l_sc", [128, L], F32)
            for b in range(L):
                S.op("act", lambda e: e.activation(out=sc[:, b:b + 1], in_=lp[:, b * 8 + 7:b * 8 + 8], func=AF.Exp, scale=-1.0), reads=[kb], writes=[kb])
            S.op("act", lambda e: e.activation(out=sc[:], in_=sc[:], func=AF.Ln, bias=1.0, scale=1.0), reads=[kb], writes=[kb])
            S.op("dve", lambda e: e.tensor_scalar(out=sc[:], in0=sc[:], scalar1=-LRU_C, scalar2=None, op0=ALU.mult), reads=[kb], writes=[kb])
            wa = T(ps, nc, "l_wa", [128, 2 * L, 128], BF16); wab = Buf("l_wa")
            for b in range(2 * L):
                S.dma("pool", wa[:, b, :], D[f"lruw_{l}"][b], writes=[wab])
            lx = T(ps, nc, "l_lx", [128, c.S], F32); lxb = Buf("l_lx")
            lg = T(ps, nc, "l_lg", [128, c.S], F32); lgb = Buf("l_lg")
            xc = T(ps, nc, "l_xc", [128, c.S], F32); xcb = Buf("l_xc")
            xc16 = T(ps, nc, "l_xc16", [128, c.S], BF16); xc16b = Buf("l_xc16")
            rr = T(ps, nc, "l_r", [128, c.S], F32); rrb = Buf("l_r")
            ig = T(ps, nc, "l_i", [128, c.S], F32); igb = Buf("l_i")
            aa, aab = rr, rrb
            uu, uub = ig, igb
            hh = T(ps, nc, "l_h", [128, c.S], F32); hhb = Buf("l_h")
            tt, ttb = lx, lxb
            C16 = T(ps, nc, "l_C", [128, c.S], BF16); Cb = Buf("l_C")
            p_r = [PS(ps, nc, f"l_pr{i}", [128, c.QR]) for i in range(2)]; p_rb = [Buf(f"l_pr{i}") for i in range(2)]
            p_i = [PS(ps, nc, f"l_pi{i}", [128, c.QR]) for i in range(2)]; p_ib = [Buf(f"l_pi{i}") for i in range(2)]
            base0 = c.RHL * 8 + c.FHL * 3
            Sn = c.S

            def SF(*a_, **k_):
                r_ = S.op(*a_, **k_)
                self.fill_hook(2)
                return r_
            for b in range(L):
                col = lambda k: lp[:, b * 8 + k:b * 8 + k + 1]
                S.dma("sp", lg[:], D["pz"][base0 + 2 * b], reads=[pzb], writes=[lgb])
                S.dma("sp", lx[:], D["pz"][base0 + 2 * b + 1], reads=[pzb], writes=[lxb])
                SF("dve", lambda e: e.tensor_scalar(out=xc[:], in0=lx[:], scalar1=col(3), scalar2=col(4), op0=ALU.mult, op1=ALU.add),
                     reads=[lxb, kb], writes=[xcb])
                for sh in (1, 2, 3):
                    SF("dve", lambda e: e.scalar_tensor_tensor(out=xc[:, sh:], in0=lx[:, :Sn - sh], scalar=col(3 - sh), in1=xc[:, sh:],
                                                                 op0=ALU.mult, op1=ALU.add), reads=[lxb, xcb, kb], writes=[xcb])
                SF("pool", lambda e: e.tensor_copy(out=xc16[:], in_=xc[:]), reads=[xcb], writes=[xc16b])
                for r in range(NR):
                    sl = slice(r * c.QR, (r + 1) * c.QR)
                    a = r % 2
                    SF("pe", lambda e: e.matmul(p_r[a][:], lhsT=wa[:, b, :], rhs=xc16[:, sl], start=True, stop=True), reads=[wab, xc16b], writes=[p_rb[a]], inc=True)
                    SF("pe", lambda e: e.matmul(p_i[a][:], lhsT=wa[:, L + b, :], rhs=xc16[:, sl], start=True, stop=True), reads=[wab, xc16b], writes=[p_ib[a]], inc=True)
                    SF("act", lambda e: e.activation(out=rr[:, sl], in_=p_r[a][:], func=AF.Sigmoid, bias=col(5), scale=1.0), reads=[p_rb[a], kb], writes=[rrb])
                    SF("act", lambda e: e.activation(out=ig[:, sl], in_=p_i[a][:], func=AF.Sigmoid, bias=col(6), scale=1.0), reads=[p_ib[a], kb], writes=[igb])
                SF("act", lambda e: e.activation(out=aa[:], in_=rr[:], func=AF.Exp, scale=sc[:, b:b + 1]), reads=[rrb, kb], writes=[aab])
                SF("pool", lambda e: e.tensor_tensor(out=tt[:], in0=aa[:], in1=aa[:], op=ALU.mult), reads=[aab], writes=[ttb])
                SF("pool", lambda e: e.tensor_scalar(out=tt[:], in0=tt[:], scalar1=-1.0, scalar2=1.0, op0=ALU.mult, op1=ALU.add), reads=[ttb], writes=[ttb])
                SF("act", lambda e: e.activation(out=tt[:], in_=tt[:], func=AF.Sqrt), reads=[ttb], writes=[ttb])
                SF("dve", lambda e: e.tensor_tensor(out=uu[:], in0=ig[:], in1=xc[:], op=ALU.mult), reads=[igb, xcb], writes=[uub])
                SF("dve", lambda e: e.tensor_tensor(out=uu[:], in0=uu[:], in1=tt[:], op=ALU.mult), reads=[uub, ttb], writes=[uub])
                SF("dve", lambda e: e.tensor_tensor_scan(out=hh[:], data0=aa[:], data1=uu[:], initial=0.0, op0=ALU.mult, op1=ALU.add),
                     reads=[aab, uub], writes=[hhb])
                SF("pool", lambda e: e.tensor_tensor(out=tt[:], in0=lg[:], in1=lg[:], op=ALU.mult), reads=[lgb], writes=[ttb])
                SF("pool", lambda e: e.tensor_scalar(out=tt[:], in0=tt[:], scalar1=0.044715, scalar2=1.0, op0=ALU.mult, op1=ALU.add), reads=[ttb], writes=[ttb])
                SF("pool", lambda e: e.tensor_tensor(out=tt[:], in0=tt[:], in1=lg[:], op=ALU.mult), reads=[ttb, lgb], writes=[ttb])
                SF("act", lambda e: e.activation(out=tt[:], in_=tt[:], func=AF.Sigmoid, scale=2.0 * math.sqrt(2.0 / math.pi)), reads=[ttb], writes=[ttb])
                SF("dve", lambda e: e.tensor_tensor(out=tt[:], in0=tt[:], in1=lg[:], op=ALU.mult), reads=[ttb, lgb], writes=[ttb])
                SF("dve", lambda e: e.tensor_tensor(out=C16[:], in0=tt[:], in1=hh[:], op=ALU.mult), reads=[ttb, hhb], writes=[Cb])
                S.dma("sp", self.abcout_ap(2, b), C16[:], reads=[Cb], writes=[abcb])
            S.barrier()

    def phase3(self, l):
        c, S, nc, D = self.cfg, self.S, self.nc, self.Dr
        gscb = Buf("gsc")
        with contextlib.ExitStack() as ps:
          if not getattr(self, "gates_done", False):
                h16 = T(ps, nc, "p3_h", [128, c.DC, c.TPC], BF16); hb = Buf("p3_h")
                for i in range(c.DC):
                    S.dma("sp", h16[:, i, :], self.hx_ap(i), writes=[hb])
                g16 = [T(ps, nc, f"p3_g{i}", [128, c.TPC], BF16) for i in range(2)]; g16b = [Buf(f"p3_g{i}") for i in range(2)]
                pp = [[PS(ps, nc, f"p3_pp{a}_{t}", [128, c.NB]) for t in range(c.NTB)] for a in range(2)]
                ppb = [[Buf(f"p3pp{a}_{t}") for t in range(c.NTB)] for a in range(2)]
                for cc in range(3 * c.DC):
                    a = cc % 2
                    w, wbf = self.wload(D[f"wmrg_{l}"][cc], c.DC * 128)
                    for t in range(c.NTB):
                        sl = slice(t * c.NB, (t + 1) * c.NB)
                        for kc in range(c.DC):
                            S.op("pe", lambda e: e.matmul(pp[a][t][:], lhsT=w[:, kc * 128:(kc + 1) * 128], rhs=h16[:, kc, sl],
                                                           start=(kc == 0), stop=(kc == c.DC - 1)),
                                 reads=[wbf, hb], writes=[ppb[a][t]], inc=(kc == c.DC - 1))
                        S.op("act", lambda e: e.activation(out=g16[a][:, sl], in_=pp[a][t][:], func=AF.Sigmoid), reads=[ppb[a][t]], writes=[g16b[a]])
                    S.dma("sp", self.gsc_ap(cc), g16[a][:], reads=[g16b[a]], writes=[gscb])
                S.barrier()
        with contextlib.ExitStack() as ps:
            y16 = T(ps, nc, "p3_y", [128, c.DC, c.TPC], BF16); yb = Buf("p3_y")
            with contextlib.ExitStack() as ps2:
                ab = T(ps2, nc, "p3_ab", [128, c.BC, c.TPC], BF16); abb = Buf("p3_ab")
                gt = [T(ps2, nc, f"p3_gt{i}", [128, c.TPC], BF16) for i in range(2)]; gtb = [Buf(f"p3_gt{i}") for i in range(2)]
                tmp = T(ps2, nc, "p3_tmp", [128, c.NB], F32); tmpb = Buf("p3_tmp")
                pp = [[PS(ps2, nc, f"p3_pm{a}_{t}", [128, c.NB]) for t in range(c.NTB)] for a in range(2)]
                ppb = [[Buf(f"p3pm{a}_{t}") for t in range(c.NTB)] for a in range(2)]
                k = 0
                for br in range(3):
                    for kk in range(c.BC):
                        S.dma("sp", ab[:, kk, :], self.abcin_ap(br, kk), writes=[abb])
                    for i in range(c.DC):
                        a = k % 2
                        k += 1
                        w, wbf = self.wload(D[f"wbr_{l}"][br * c.DC + i], c.BC * 128)
                        S.dma("sp", gt[a][:], self.gsc_ap(br * c.DC + i), reads=[gscb], writes=[gtb[a]])
                        for t in range(c.NTB):
                            sl = slice(t * c.NB, (t + 1) * c.NB)
                            for kk in range(c.BC):
                                S.op("pe", lambda e: e.matmul(pp[a][t][:], lhsT=w[:, kk * 128:(kk + 1) * 128], rhs=ab[:, kk, sl],
                                                               start=(kk == 0), stop=(kk == c.BC - 1)),
                                     reads=[wbf, abb], writes=[ppb[a][t]], inc=(kk == c.BC - 1))
                            if br == 0:
                                S.op("dve", lambda e: e.tensor_tensor(out=y16[:, i, sl], in0=pp[a][t][:], in1=gt[a][:, sl], op=ALU.mult),
                                     reads=[ppb[a][t], gtb[a]], writes=[yb])
                            else:
                                S.op("dve", lambda e: e.tensor_tensor(out=tmp[:], in0=pp[a][t][:], in1=gt[a][:, sl], op=ALU.mult),
                                     reads=[ppb[a][t], gtb[a]], writes=[tmpb])
                                S.op("dve", lambda e: e.tensor_tensor(out=y16[:, i, sl], in0=y16[:, i, sl], in1=tmp[:], op=ALU.add),
                                     reads=[tmpb, yb], writes=[yb])
                S.barrier()
            with contextlib.ExitStack() as ps2:
                pp = [[PS(ps2, nc, f"p3_po{a}_{t}", [128, c.NB]) for t in range(c.NTB)] for a in range(2)]
                ppb = [[Buf(f"p3po{a}_{t}") for t in range(c.NTB)] for a in range(2)]
                for i in range(c.DC):
                    a = i % 2
                    w, wbf = self.wload(D[f"wout_{l}"][i], c.DC * 128)
                    xt, xb = self.xslot()
                    S.dma("sp", xt[:], self.xs_ap(i), reads=[self.xsb[i]], writes=[xb])
                    for t in range(c.NTB):
                        sl = slice(t * c.NB, (t + 1) * c.NB)
                        for kc in range(c.DC):
                            S.op("pe", lambda e: e.matmul(pp[a][t][:], lhsT=w[:, kc * 128:(kc + 1) * 128], rhs=y16[:, kc, sl],
                                                           start=(kc == 0), stop=(kc == c.DC - 1)),
                                 reads=[wbf, yb], writes=[ppb[a][t]], inc=(kc == c.DC - 1))
                        S.op("dve", lambda e: e.tensor_tensor(out=xt[:, sl], in0=pp[a][t][:], in1=xt[:, sl], op=ALU.add),
                             reads=[ppb[a][t], xb], writes=[xb])
                    S.dma("sp", self.xs_ap(i), xt[:], reads=[xb], writes=[self.xsb[i]])
                S.barrier()


def pretile(W):
    K, M = W.shape
    return np.ascontiguousarray(W.reshape(K // 128, 128, M // 128, 128).transpose(2, 1, 0, 3)).reshape(M // 128, 128, K)


def vec_fm(v):
    return np.ascontiguousarray(v.reshape(-1, 128).T)


def consts(cfg, g):
    c = cfg
    out = {}
    half = 128
    inv_freq = ROPE_BASE ** (-np.arange(half, dtype=np.float32) / half)
    ang = np.arange(c.S, dtype=np.float32)[None, :] * inv_freq[:, None]
    out["cs"] = np.stack([np.cos(ang), np.sin(ang)]).astype(np.float32)
    heads = np.arange(g * c.RHL, (g + 1) * c.RHL)
    lg = np.log(1.0 - np.exp2(-5.0 - heads.astype(np.float32))).astype(np.float32)
    idx = np.arange(128, dtype=np.float32)
    rel = idx[None, :] - idx[:, None]
    sc = np.float32(256.0 ** -0.5)
    rmask = np.where(rel[None] >= 0, np.exp(np.maximum(rel, 0)[None] * lg[:, None, None]), 0.0) * sc
    out["rmask"] = rmask.astype(np.float32)
    rvec = np.zeros((128, c.RHL * 3), np.float32)
    for h in range(c.RHL):
        rvec[:, 3 * h] = np.exp((127.0 - idx) * lg[h]) * sc
        rvec[:, 3 * h + 1] = np.exp((idx + 1.0) * lg[h])
        rvec[:, 3 * h + 2] = np.exp(128.0 * lg[h])
    out["rvec"] = rvec
    tri = (idx[None, :] >= idx[:, None]).astype(np.float32)
    cb = np.concatenate([tri, np.eye(128, dtype=np.float32), np.ones((128, 128), np.float32)], axis=1)
    out["cb16"] = cb.astype(NPBF)
    sel = np.zeros((c.FHL, c.FHL, 128), np.float32)
    for h in range(c.FHL):
        sel[h, h, :] = 1.0
    out["sel"] = sel.reshape(c.FHL, c.FHL * 128).astype(NPBF)
    out["id8"] = np.eye(c.FHL, dtype=np.float32)
    return out


def mix_cols(cfg, g):
    c = cfg
    o_rq, o_rk, o_rv, o_rg = 0, c.RW, 2 * c.RW, 3 * c.RW
    o_fq = 4 * c.RW
    o_fk, o_fv = o_fq + c.FW, o_fq + 2 * c.FW
    o_fg = o_fq + 3 * c.FW
    o_lg = o_fg + c.FH
    o_lx = o_lg + c.LW
    o_m = o_lx + c.LW
    cols = []
    for hl in range(c.RHL):
        h = g * c.RHL + hl
        for o in (o_rq, o_rk, o_rv, o_rg):
            cols.append(np.arange(o + h * 256, o + (h + 1) * 256))
    for hl in range(c.FHL):
        h = g * c.FHL + hl
        for o in (o_fq, o_fk, o_fv):
            cols.append(np.arange(o + h * 128, o + (h + 1) * 128))
    for bl in range(c.LBL):
        b = g * c.LBL + bl
        for o in (o_lg, o_lx):
            cols.append(np.arange(o + b * 128, o + (b + 1) * 128))
    cols = np.concatenate(cols)
    fcols = np.arange(o_fg + g * c.FHL, o_fg + (g + 1) * c.FHL)
    mcols = np.arange(o_m, o_m + 3 * c.D)
    return cols, fcols, mcols


def ffn_inputs(cfg, p, inp, name, l):
    c = cfg
    d = {}
    d[p + "_n"] = vec_fm(inp[f"{name}_norm"][l])
    d[p + "_wg"] = pretile(inp[f"{name}_w_gate"][l])
    d[p + "_wu"] = pretile(inp[f"{name}_w_up"][l])
    Wd = inp[f"{name}_w_down"][l]
    d[p + "_wd"] = np.ascontiguousarray(
        Wd.reshape(c.FQ, c.AG, 128, c.DC, 128).transpose(0, 3, 2, 1, 4)).reshape(c.FQ * c.DC, 128, c.AG * 128)
    return d


_CACHE = {}


def run_chain(cfg, inp, dbg=None):
    c = cfg
    ncores = 2 * c.B
    x = inp["x"]
    ones32 = np.ones((128, 128), np.float32)
    xs = []
    for core in range(ncores):
        b, g = core // 2, core % 2
        xt = x[b, g * c.TPC:(g + 1) * c.TPC, :]
        xs.append(np.ascontiguousarray(xt.T).reshape(c.DC, 128, c.TPC))
    K = [consts(c, g) for g in range(2)]
    for l in range(c.DEPTH):
        xs, hx = launch_tok(c, "p1", l, inp, xs, None, None, ones32)
        if dbg is not None:
            dbg[f"x1_{l}"] = xs
            dbg[f"hx_{l}"] = hx
        prog = get_prog(c, (("p2", 0),))
        maps = []
        for core in range(ncores):
            b, g = core // 2, core % 2
            cols, fcols, mcols = mix_cols(c, g)
            W = inp["w_in"][l]
            m = {"hfull": np.stack([hx[2 * b], hx[2 * b + 1]]), "ones32": ones32}
            m["wmix_0"] = pretile(np.ascontiguousarray(W[:, cols]))
            m["wfg_0"] = np.ascontiguousarray(W[:, fcols].reshape(c.DC, 128, c.FHL).transpose(1, 0, 2)).reshape(128, c.DC * c.FHL)
            rn = inp["ret_norm"][l][g * c.RHL:(g + 1) * c.RHL]
            m["retgn_0"] = np.ascontiguousarray(rn.reshape(c.RHL * 2, 128).T)
            m["foxg_0"] = np.ascontiguousarray(np.stack([inp["fox_q_norm"][l], inp["fox_k_norm"][l]], axis=1))
            m["foxb_0"] = np.ascontiguousarray(inp["fox_f_bias"][l][g * c.FHL:(g + 1) * c.FHL].reshape(c.FHL, 1))
            lp = np.zeros((128, c.LBL, 8), np.float32)
            for bl in range(c.LBL):
                sl = slice((g * c.LBL + bl) * 128, (g * c.LBL + bl + 1) * 128)
                lp[:, bl, 0:4] = inp["lru_conv_w"][l][:, sl].T
                lp[:, bl, 4] = inp["lru_conv_b"][l][sl]
                lp[:, bl, 5] = inp["lru_b_a"][l][sl]
                lp[:, bl, 6] = inp["lru_b_x"][l][sl]
                lp[:, bl, 7] = inp["lru_lambda"][l][sl]
            m["lrup_0"] = lp.reshape(128, c.LBL * 8)
            m["lruw_0"] = np.ascontiguousarray(np.concatenate([inp["lru_w_a"][l][g * c.LBL:(g + 1) * c.LBL],
                                                               inp["lru_w_x"][l][g * c.LBL:(g + 1) * c.LBL]]))
            m.update(K[g])
            maps.append(m)
        res = run_bass_kernel_spmd(prog.nc, maps, core_ids=list(range(ncores)))
        abc = [r["abc_out"] for r in res.results]
        if dbg is not None:
            dbg[f"abc_{l}"] = abc
        abc_in = []
        for core in range(ncores):
            b, g = core // 2, core % 2
            tsl = slice(g * c.TPC, (g + 1) * c.TPC)
            parts = [abc[2 * b + gg][:, :, :, tsl] for gg in range(2)]
            abc_in.append(np.ascontiguousarray(np.concatenate(parts, axis=1)))
        xs, _ = launch_tok(c, "p3", l, inp, xs, hx, abc_in, ones32)
        if dbg is not None:
            dbg[f"x3_{l}"] = xs
    out = np.empty((c.B, c.S, c.D), np.float32)
    for core in range(ncores):
        b, g = core // 2, core % 2
        out[b, g * c.TPC:(g + 1) * c.TPC, :] = xs[core].reshape(c.D, c.TPC).T
    return out


def get_prog(cfg, segs):
    key = (id(cfg), segs)
    if key not in _CACHE:
        p = Prog(cfg, list(segs), None)
        p.build()
        _CACHE[key] = p
    return _CACHE[key]


def launch_tok(c, kind, l, inp, xs, hx, abc_in, ones32):
    prog = get_prog(c, ((kind, 0),))
    ncores = 2 * c.B
    shared = {"ones32": ones32}
    if kind == "p1":
        shared.update(ffn_inputs(c, "f1_0", inp, "ffn1", l))
        shared["n_mix_0"] = vec_fm(inp["mix_norm"][l])
    else:
        _, _, mcols = mix_cols(c, 0)
        shared["wmrg_0"] = pretile(np.ascontiguousarray(inp["w_in"][l][:, mcols]))
        shared["wbr_0"] = np.concatenate([pretile(inp[n][l]) for n in ("w_branch_ret", "w_branch_fox", "w_branch_lru")])
        shared["wout_0"] = pretile(inp["w_out"][l])
        shared.update(ffn_inputs(c, "f2_0", inp, "ffn2", l))
    maps = []
    for core in range(ncores):
        m = dict(shared)
        m["x_in"] = xs[core]
        if kind == "p3":
            m["abc_in"] = abc_in[core]
            m["hx"] = hx[core]
        maps.append(m)
    res = run_bass_kernel_spmd(prog.nc, maps, core_ids=list(range(ncores)))
    xs2 = [r["xs"] for r in res.results]
    hx2 = [r["hx"] for r in res.results] if kind == "p1" else hx
    return xs2, hx2


def run_fused(cfg, inp):
    c = cfg
    key = ("full", id(cfg))
    if key not in _CACHE:
        p = Prog(cfg, [], None)
        p.build_full()
        _CACHE[key] = p
    prog = _CACHE[key]
    x = inp["x"]
    shared = {"ones32": np.ones((128, 128), np.float32)}
    shared.update(consts(c, 0))
    cols, fcols, mcols = mix_cols(c, 0)
    for l in range(c.DEPTH):
        shared.update(ffn_inputs(c, f"f1_{l}", inp, "ffn1", l))
        shared.update(ffn_inputs(c, f"f2_{l}", inp, "ffn2", l))
        shared[f"n_mix_{l}"] = vec_fm(inp["mix_norm"][l])
        W = inp["w_in"][l]
        shared[f"wmix_{l}"] = pretile(np.ascontiguousarray(W[:, cols]))
        shared[f"wfg_{l}"] = np.ascontiguousarray(W[:, fcols].reshape(c.DC, 128, c.FHL).transpose(1, 0, 2)).reshape(128, c.DC * c.FHL)
        shared[f"retgn_{l}"] = np.ascontiguousarray(inp["ret_norm"][l].reshape(c.RHL * 2, 128).T)
        shared[f"foxg_{l}"] = np.ascontiguousarray(np.stack([inp["fox_q_norm"][l], inp["fox_k_norm"][l]], axis=1))
        shared[f"foxb_{l}"] = np.ascontiguousarray(inp["fox_f_bias"][l].reshape(c.FHL, 1))
        lp = np.zeros((128, c.LBL, 8), np.float32)
        for bl in range(c.LBL):
            sl = slice(bl * 128, (bl + 1) * 128)
            lp[:, bl, 0:4] = inp["lru_conv_w"][l][:, sl].T
            lp[:, bl, 4] = inp["lru_conv_b"][l][sl]
            lp[:, bl, 5] = inp["lru_b_a"][l][sl]
            lp[:, bl, 6] = inp["lru_b_x"][l][sl]
            lp[:, bl, 7] = inp["lru_lambda"][l][sl]
        shared[f"lrup_{l}"] = lp.reshape(128, c.LBL * 8)
        shared[f"lruw_{l}"] = np.ascontiguousarray(np.concatenate([inp["lru_w_a"][l], inp["lru_w_x"][l]]))
        shared[f"wmrg_{l}"] = pretile(np.ascontiguousarray(W[:, mcols]))
        shared[f"wbr_{l}"] = np.concatenate([pretile(inp[n][l]) for n in ("w_branch_ret", "w_branch_fox", "w_branch_lru")])
        shared[f"wout_{l}"] = pretile(inp["w_out"][l])
    spread = os.environ.get("SPREAD", "1") == "1" and c.B == 4
    real = [0, 1, 4, 5] if spread else list(range(c.B))
    ncore = 8 if spread else c.B
    zero = {k: np.zeros_like(v) for k, v in shared.items()} if spread else None
    maps = []
    for core in range(ncore):
        if core in real:
            b = real.index(core)
            m = dict(shared)
            m["x_in"] = np.ascontiguousarray(x[b].T).reshape(c.DC, 128, 2, c.TPC).transpose(2, 0, 1, 3).copy()
        else:
            m = dict(zero)
            m["x_in"] = np.zeros((2, c.DC, 128, c.TPC), np.float32)
        maps.append(m)
    res = run_bass_kernel_spmd(prog.nc, maps, core_ids=list(range(ncore)))
    out = np.empty((c.B, c.S, c.D), np.float32)
    for b in range(c.B):
        xs = res.results[real[b]]["xs"]
        out[b] = xs.transpose(0, 3, 1, 2).reshape(c.S, c.D)
    return out


def kernel(**inputs):
    cfg = _CACHE.setdefault("cfg", CFG(full=True))
    inp = {k: np.asarray(v) for k, v in inputs.items()}
    return run_fused(cfg, inp)
```
